# Optimizing a Trainium2 kernel written in Bass

```python
import math
import jax, jax.numpy as jnp
from jax import lax
import numpy as np

D_MODEL = 1024
BATCH = 2
SEQ = 8192
DEPTH = 2

GRID_W = 64
CTX_LEN = 256
EPS = 1e-6
ROPE_BASE = 10000.0
ATTN_BLOCK = 128

A_HEADS = 4
A_DK = 128
A_DV = 128
A_WIDTH = A_HEADS * A_DK
HGRN_CHUNK = 16
B_HEADS = 4
B_HALF = 64
B_DV = 2 * B_HALF
B_WIDTH = B_HEADS * B_DV
EVEN_IN = 5 * A_WIDTH + 3 * B_WIDTH
C_HEADS = 16
C_KV_HEADS = 4
C_HEAD_DIM = 64
C_WINDOW = 128
ODD_IN = (C_HEADS + 2 * C_KV_HEADS) * C_HEAD_DIM
N_EXPERTS = 16
N_GROUPS = 4
EXPERTS_PER_GROUP = N_EXPERTS // N_GROUPS
TOP_K = 2
D_EXPERT = 1024
MOE_BLOCK = 128

N_EVEN = (DEPTH + 1) // 2
N_ODD = DEPTH // 2

kernel_name = 'hybrid_hgrn2_diffattn_swa_groupmoe_dit'

F32 = jnp.float32


def rms_norm(x, g):
    xf = x.astype(F32)
    y = xf * lax.rsqrt(jnp.mean(xf * xf, axis=-1, keepdims=True) + EPS)
    return (y * g.astype(F32)).astype(x.dtype)


def axial_rope_tables(n_tokens, dim):
    n_rows = n_tokens // GRID_W
    row = jnp.repeat(jnp.arange(n_rows), GRID_W).astype(F32)
    col = jnp.tile(jnp.arange(GRID_W), n_rows).astype(F32)
    half = dim // 2
    inv = 1.0 / (ROPE_BASE ** (jnp.arange(0, half, 2, dtype=F32) / half))
    ar = row[:, None] * inv
    ac = col[:, None] * inv
    ang = jnp.concatenate([ar, ar, ac, ac], axis=-1)
    return jnp.cos(ang), jnp.sin(ang)


def _rotate_half(x):
    x1, x2 = jnp.split(x, 2, axis=-1)
    return jnp.concatenate([-x2, x1], axis=-1)


def apply_axial_rope(x, cos, sin):
    xr, xc = jnp.split(x, 2, axis=-1)
    xrot = jnp.concatenate([_rotate_half(xr), _rotate_half(xc)], axis=-1)
    return (x * cos[None, :, None, :] + xrot * sin[None, :, None, :]).astype(x.dtype)


def hgrn2_scan(q, k, v, logf, s0):
    b_, t_, h_, _ = q.shape
    dv = v.shape[-1]
    n = t_ // HGRN_CHUNK

    def chunks(a):
        return a.astype(F32).reshape(b_, n, HGRN_CHUNK, h_, a.shape[-1])

    qc, kc, vc, lf = chunks(q), chunks(k), chunks(v), chunks(logf)
    cum = jnp.cumsum(lf, axis=2)
    total = cum[:, :, -1]
    tri = jnp.tril(jnp.ones((HGRN_CHUNK, HGRN_CHUNK), dtype=bool))
    diff = cum[:, :, :, None] - cum[:, :, None, :]
    decay = jnp.exp(jnp.where(tri[:, :, None, None], diff, -jnp.inf))
    scores = jnp.einsum('bnthk,bntshk,bnshk->bnhts', qc, decay, kc)
    o_intra = jnp.einsum('bnhts,bnshv->bnthv', scores, vc)
    q_in = qc * jnp.exp(cum)
    k_out = kc * jnp.exp(total[:, :, None] - cum)

    def step(state, inp):
        q_n, k_n, v_n, tot_n = inp
        o_n = jnp.einsum('bthk,bhkv->bthv', q_n, state)
        state = state * jnp.exp(tot_n)[..., None] + jnp.einsum('bshk,bshv->bhkv', k_n, v_n)
        return state, o_n

    xs = (jnp.moveaxis(q_in, 1, 0), jnp.moveaxis(k_out, 1, 0), jnp.moveaxis(vc, 1, 0), jnp.moveaxis(total, 1, 0))
    s_final, o_inter = lax.scan(step, s0, xs)
    o = o_intra + jnp.moveaxis(o_inter, 0, 1)
    return o.reshape(b_, t_, h_, dv), s_final


def hgrn2_mixer(p, pc, lb, onorm_g):
    b_ = p.shape[0]

    def heads(a):
        return a.reshape(a.shape[0], a.shape[1], A_HEADS, -1)

    def split(a):
        q, ff, fb, i, g = jnp.split(a, 5, axis=-1)
        q = heads(jax.nn.silu(q.astype(F32)) * (A_DK ** -0.5))
        return q, heads(ff), heads(fb), heads(i), heads(g)

    ql, ffl, fbl, il, gl = split(p)
    qc, ffc, fbc, ic, gc = split(pc)
    lb = lb.astype(F32).reshape(2, A_HEADS, A_DK)

    def gates(f_raw, lb_d):
        f = lb_d + (1.0 - lb_d) * jax.nn.sigmoid(f_raw.astype(F32))
        return 1.0 - f, jnp.log(f)

    def direction(d, reverse, f_lat, f_ctx):
        fl = (lambda a: jnp.flip(a, axis=1)) if reverse else (lambda a: a)
        k_c, lf_c = gates(f_ctx, lb[d])
        k_l, lf_l = gates(f_lat, lb[d])
        s0 = jnp.zeros((b_, A_HEADS, A_DK, A_DV), F32)
        o_c, s_c = hgrn2_scan(fl(qc), fl(k_c), fl(ic), fl(lf_c), s0)
        o_l, _ = hgrn2_scan(fl(ql), fl(k_l), fl(il), fl(lf_l), s_c)
        return fl(o_c), fl(o_l)

    oc_f, ol_f = direction(0, False, ffl, ffc)
    oc_b, ol_b = direction(1, True, fbl, fbc)

    def finish(o, g):
        y = rms_norm(o, onorm_g) * jax.nn.silu(g.astype(F32))
        return y.reshape(y.shape[0], y.shape[1], A_WIDTH)

    return finish(ol_f + ol_b, gl), finish(oc_f + oc_b, gc)


def diff_attention_mixer(p, pc, lam_p, subln_g, lambda_init, rope, need_ctx):
    b_, t_, _ = p.shape

    def split_qkv(a):
        bb, tt = a.shape[0], a.shape[1]
        q, k, v = jnp.split(a, 3, axis=-1)
        return (q.reshape(bb, tt, B_HEADS, 2, B_HALF), k.reshape(bb, tt, B_HEADS, 2, B_HALF),
                v.reshape(bb, tt, B_HEADS, B_DV))

    q, k, v = split_qkv(p)
    qc, kc, vc = split_qkv(pc)
    cos, sin = rope
    q = apply_axial_rope(q.reshape(b_, t_, 2 * B_HEADS, B_HALF), cos, sin).reshape(b_, t_, B_HEADS, 2, B_HALF)
    k = apply_axial_rope(k.reshape(b_, t_, 2 * B_HEADS, B_HALF), cos, sin).reshape(b_, t_, B_HEADS, 2, B_HALF)
    lam_p = lam_p.astype(F32)
    lam = jnp.exp(jnp.sum(lam_p[0] * lam_p[1])) - jnp.exp(jnp.sum(lam_p[2] * lam_p[3])) + lambda_init
    scale = B_HALF ** -0.5

    def attend(qb, keys, vals):
        s = jnp.einsum('bqhmd,bkhmd->bhmqk', qb, keys).astype(F32) * scale
        pm = jax.nn.softmax(s, axis=-1)
        a = pm[:, :, 0] - lam * pm[:, :, 1]
        return jnp.einsum('bhqk,bkhv->bqhv', a, vals)

    keys = jnp.concatenate([kc, k], axis=1)
    vals = jnp.concatenate([vc, v], axis=1)
    nb = t_ // ATTN_BLOCK
    qb = jnp.moveaxis(q.reshape(b_, nb, ATTN_BLOCK, B_HEADS, 2, B_HALF), 1, 0)
    o = lax.map(lambda blk: attend(blk, keys, vals), qb)
    o = jnp.moveaxis(o, 0, 1).reshape(b_, t_, B_HEADS, B_DV)

    def finish(o):
        y = rms_norm(o, subln_g) * (1.0 - lambda_init)
        return y.reshape(y.shape[0], y.shape[1], B_WIDTH)

    y = finish(o)
    yc = finish(attend(qc, kc, vc)) if need_ctx else None
    return y, yc


def even_layer_mixer(h, hc, w_in, w_out, lb, onorm_g, lam_p, subln_g, lambda_init, rope, need_ctx):
    p = h @ w_in
    pc = hc @ w_in
    ya, yac = hgrn2_mixer(p[..., :5 * A_WIDTH], pc[..., :5 * A_WIDTH], lb, onorm_g)
    yb, ybc = diff_attention_mixer(p[..., 5 * A_WIDTH:], pc[..., 5 * A_WIDTH:], lam_p, subln_g, lambda_init, rope, need_ctx)
    y = jnp.concatenate([ya, yb], axis=-1) @ w_out
    yc = (jnp.concatenate([yac, ybc], axis=-1) @ w_out) if need_ctx else None
    return y, yc


def _sink_attend(qb, parts, sink_logit):
    scale = C_HEAD_DIM ** -0.5
    logits = []
    for kk, _, valid in parts:
        s = jnp.einsum('bqkgd,bskd->bkgqs', qb, kk).astype(F32) * scale
        if valid is not None:
            s = jnp.where(valid, s, -jnp.inf)
        logits.append(s)
    b_, nq = qb.shape[0], qb.shape[1]
    sink_col = jnp.broadcast_to(sink_logit[None, :, :, None, None], (b_, C_KV_HEADS, C_HEADS // C_KV_HEADS, nq, 1))
    pr = jax.nn.softmax(jnp.concatenate([sink_col] + logits, axis=-1), axis=-1)
    out = 0.0
    off = 1
    for (_, vv, _), s in zip(parts, logits):
        n = s.shape[-1]
        out = out + jnp.einsum('bkgqs,bskd->bqkgd', pr[..., off:off + n], vv)
        off += n
    return out


def odd_layer_mixer(h, hc, w_qkv, w_out, sink, rope, need_ctx):
    b_, t_, d_ = h.shape
    n_ctx = hc.shape[1]
    grp = C_HEADS // C_KV_HEADS
    q_cols = C_HEADS * C_HEAD_DIM
    p = h @ w_qkv
    q = p[..., :q_cols].reshape(b_, t_, C_HEADS, C_HEAD_DIM)
    k, v = jnp.split(p[..., q_cols:], 2, axis=-1)
    k = k.reshape(b_, t_, C_KV_HEADS, C_HEAD_DIM)
    v = v.reshape(b_, t_, C_KV_HEADS, C_HEAD_DIM)
    if need_ctx:
        pc = hc @ w_qkv
        qc = pc[..., :q_cols].reshape(b_, n_ctx, C_KV_HEADS, grp, C_HEAD_DIM)
        kvc = pc[..., q_cols:]
    else:
        kvc = hc @ w_qkv[:, q_cols:]
    kc, vc = jnp.split(kvc, 2, axis=-1)
    kc = kc.reshape(b_, n_ctx, C_KV_HEADS, C_HEAD_DIM)
    vc = vc.reshape(b_, n_ctx, C_KV_HEADS, C_HEAD_DIM)
    cos, sin = rope
    q = apply_axial_rope(q, cos, sin).reshape(b_, t_, C_KV_HEADS, grp, C_HEAD_DIM)
    k = apply_axial_rope(k, cos, sin)
    sink_logit = sink.astype(F32).reshape(C_KV_HEADS, grp)
    pad = ((0, 0), (C_WINDOW, C_WINDOW), (0, 0), (0, 0))
    kp = jnp.pad(k, pad)
    vp = jnp.pad(v, pad)
    span = ATTN_BLOCK + 2 * C_WINDOW
    nb = t_ // ATTN_BLOCK

    def block(n):
        start = n * ATTN_BLOCK
        qb = lax.dynamic_slice_in_dim(q, start, ATTN_BLOCK, axis=1)
        kb = lax.dynamic_slice_in_dim(kp, start, span, axis=1)
        vb = lax.dynamic_slice_in_dim(vp, start, span, axis=1)
        qpos = start + jnp.arange(ATTN_BLOCK)
        kpos = start - C_WINDOW + jnp.arange(span)
        valid = (jnp.abs(qpos[:, None] - kpos[None, :]) <= C_WINDOW) & (kpos[None, :] >= 0) & (kpos[None, :] < t_)
        return _sink_attend(qb, [(kc, vc, None), (kb, vb, valid)], sink_logit)

    o = lax.map(block, jnp.arange(nb))
    y = jnp.moveaxis(o, 0, 1).reshape(b_, t_, d_) @ w_out
    yc = None
    if need_ctx:
        yc = _sink_attend(qc, [(kc, vc, None)], sink_logit).reshape(b_, n_ctx, d_) @ w_out
    return y, yc


def routed_moe(h2d, router_w, router_b, w_gate, w_up, w_down):
    n_tok, d_ = h2d.shape
    aff = jax.nn.sigmoid(jnp.dot(h2d, router_w).astype(F32))
    sel = (aff + router_b.astype(F32)).reshape(n_tok, N_GROUPS, EXPERTS_PER_GROUP)
    group_score = jnp.sum(lax.top_k(sel, TOP_K)[0], axis=-1)
    grp = jnp.argmax(group_score, axis=-1)
    in_group = jnp.take_along_axis(sel, grp[:, None, None], axis=1)[:, 0]
    _, local = lax.top_k(in_group, TOP_K)
    expert = (grp[:, None] * EXPERTS_PER_GROUP + local).astype(jnp.int32)
    wts = jnp.take_along_axis(aff, expert, axis=1)
    wts = wts / jnp.sum(wts, axis=-1, keepdims=True)
    n_assign = n_tok * TOP_K
    n_blocks = -(-n_assign // MOE_BLOCK) + N_EXPERTS
    n_rows = n_blocks * MOE_BLOCK
    flat_e = expert.reshape(-1)
    flat_t = jnp.repeat(jnp.arange(n_tok, dtype=jnp.int32), TOP_K)
    flat_w = wts.reshape(-1)
    order = jnp.argsort(flat_e)
    e_sorted = flat_e[order]
    counts = jnp.bincount(flat_e, length=N_EXPERTS)
    padded = (counts + MOE_BLOCK - 1) // MOE_BLOCK * MOE_BLOCK
    pad_end = jnp.cumsum(padded)
    pad_start = pad_end - padded
    start = jnp.cumsum(counts) - counts
    dest = pad_start[e_sorted] + jnp.arange(n_assign) - start[e_sorted]
    row_tok = jnp.full((n_rows,), n_tok, jnp.int32).at[dest].set(flat_t[order])
    row_w = jnp.zeros((n_rows,), F32).at[dest].set(flat_w[order])
    block_start = jnp.arange(n_blocks) * MOE_BLOCK
    block_e = jnp.minimum(jnp.sum(pad_end[None, :] <= block_start[:, None], axis=1), N_EXPERTS - 1)
    x_pad = jnp.concatenate([h2d, jnp.zeros((1, d_), h2d.dtype)], axis=0)
    xb = x_pad[row_tok].reshape(n_blocks, MOE_BLOCK, d_)

    def expert_block(args):
        xblk, e = args
        hid = jax.nn.silu(xblk @ w_gate[e]) * (xblk @ w_up[e])
        return hid @ w_down[e]

    yb = lax.map(expert_block, (xb, block_e)).reshape(n_rows, d_)
    out = jnp.zeros((n_tok + 1, d_), F32).at[row_tok].add(yb * row_w[:, None])
    return out[:n_tok]


def setup_inputs(seed: int = 0) -> dict:
    key = jax.random.key(seed)
    ks = jax.random.split(key, 24)
    d = D_MODEL
    nrm = jax.random.normal
    return {
        'x': nrm(ks[0], (BATCH, SEQ, d), F32),
        'c': nrm(ks[1], (BATCH, d), F32),
        'ctx': nrm(ks[2], (BATCH, CTX_LEN, d), F32),
        'c_ctx': nrm(ks[3], (d,), F32),
        'ada_w': nrm(ks[4], (DEPTH, d, 6 * d), F32) * (0.5 * d ** -0.5),
        'ada_b': nrm(ks[5], (DEPTH, 6 * d), F32) * 0.02,
        'norm_mix_g': 1.0 + 0.02 * nrm(ks[6], (DEPTH, d), F32),
        'norm_ffn_g': 1.0 + 0.02 * nrm(ks[7], (DEPTH, d), F32),
        'even_w_in': nrm(ks[8], (N_EVEN, d, EVEN_IN), F32) * d ** -0.5,
        'even_w_out': nrm(ks[9], (N_EVEN, A_WIDTH + B_WIDTH, d), F32) * (A_WIDTH + B_WIDTH) ** -0.5,
        'hgrn_lb_logits': 0.1 * nrm(ks[10], (DEPTH + 1, 2, A_WIDTH), F32),
        'hgrn_onorm_g': 1.0 + 0.02 * nrm(ks[11], (N_EVEN, A_DV), F32),
        'diff_lambda': 0.1 * nrm(ks[12], (N_EVEN, 4, B_HALF), F32),
        'diff_subln_g': 1.0 + 0.02 * nrm(ks[13], (N_EVEN, B_DV), F32),
        'odd_w_qkv': nrm(ks[14], (N_ODD, d, ODD_IN), F32) * d ** -0.5,
        'odd_w_out': nrm(ks[15], (N_ODD, C_HEADS * C_HEAD_DIM, d), F32) * (C_HEADS * C_HEAD_DIM) ** -0.5,
        'swa_sink': 0.5 * nrm(ks[16], (N_ODD, C_HEADS), F32),
        'router_w': nrm(ks[17], (d, N_EXPERTS), F32) * d ** -0.5,
        'router_b': 0.01 * nrm(ks[18], (N_EXPERTS,), F32),
        'moe_w_gate': nrm(ks[19], (DEPTH, N_EXPERTS, d, D_EXPERT), F32) * d ** -0.5,
        'moe_w_up': nrm(ks[20], (DEPTH, N_EXPERTS, d, D_EXPERT), F32) * d ** -0.5,
        'moe_w_down': nrm(ks[21], (DEPTH, N_EXPERTS, D_EXPERT, d), F32) * D_EXPERT ** -0.5,
        'final_norm_g': 1.0 + 0.02 * nrm(ks[22], (d,), F32),
    }


def reference(x, c, ctx, c_ctx, ada_w, ada_b, norm_mix_g, norm_ffn_g, even_w_in, even_w_out,
              hgrn_lb_logits, hgrn_onorm_g, diff_lambda, diff_subln_g, odd_w_qkv, odd_w_out, swa_sink,
              router_w, router_b, moe_w_gate, moe_w_up, moe_w_down, final_norm_g):
    b_, t_, d_ = x.shape
    n_ctx = ctx.shape[1]
    rope_b = axial_rope_tables(t_, B_HALF)
    rope_c = axial_rope_tables(t_, C_HEAD_DIM)
    lower_bounds = jnp.cumsum(jax.nn.softmax(hgrn_lb_logits.astype(F32), axis=0), axis=0)
    xc = ctx
    for layer in range(DEPTH):
        last = layer == DEPTH - 1
        w_ada, b_ada = ada_w[layer], ada_b[layer]
        mod = (jax.nn.silu(c) @ w_ada + b_ada)[:, None, :]
        mod_c = (jax.nn.silu(c_ctx) @ w_ada + b_ada)[None, None, :]
        sh1, sc1, g1, sh2, sc2, g2 = jnp.split(mod, 6, axis=-1)
        csh1, csc1, cg1, csh2, csc2, cg2 = jnp.split(mod_c, 6, axis=-1)
        h = rms_norm(x, norm_mix_g[layer]) * (1.0 + sc1) + sh1
        hc = rms_norm(xc, norm_mix_g[layer]) * (1.0 + csc1) + csh1
        if layer % 2 == 0:
            j = layer // 2
            lambda_init = 0.8 - 0.6 * math.exp(-0.3 * layer)
            y, yc = even_layer_mixer(h, hc, even_w_in[j], even_w_out[j], lower_bounds[layer], hgrn_onorm_g[j],
                                     diff_lambda[j], diff_subln_g[j], lambda_init, rope_b, not last)
        else:
            j = layer // 2
            y, yc = odd_layer_mixer(h, hc, odd_w_qkv[j], odd_w_out[j], swa_sink[j], rope_c, not last)
        x = x + g1 * y
        h2 = rms_norm(x, norm_ffn_g[layer]) * (1.0 + sc2) + sh2
        if last:
            f = routed_moe(h2.reshape(-1, d_), router_w, router_b,
                           moe_w_gate[layer], moe_w_up[layer], moe_w_down[layer])
            x = x + g2 * f.reshape(b_, t_, d_)
        else:
            xc = xc + cg1 * yc
            hc2 = rms_norm(xc, norm_ffn_g[layer]) * (1.0 + csc2) + csh2
            tokens = jnp.concatenate([h2.reshape(-1, d_), hc2.reshape(-1, d_)], axis=0)
            f = routed_moe(tokens, router_w, router_b, moe_w_gate[layer], moe_w_up[layer], moe_w_down[layer])
            x = x + g2 * f[:b_ * t_].reshape(b_, t_, d_)
            xc = xc + cg2 * f[b_ * t_:].reshape(b_, n_ctx, d_)
    return rms_norm(x, final_norm_g)
```

```python
import math
import numpy as np
import concourse.bass as bass
import concourse.mybir as mybir
from concourse.bass_utils import run_bass_kernel_spmd
from contextlib import ExitStack

F32 = mybir.dt.float32
BF16 = mybir.dt.bfloat16
AF = mybir.ActivationFunctionType
ALU = mybir.AluOpType
AX = mybir.AxisListType
ENG = ['pe', 'act', 'dve', 'pool', 'sp']

D = 1024
KT = 8
NCTX = 256
EPS = 1e-6
NE = 16
HC = 64


class Sched:
    def __init__(self, nc, es):
        self.nc = nc
        self.es = es
        self.ops = []
        self.sems = {}
        self.bars = []

    def add(self, eng, fn, r=(), w=(), dsem=None):
        self.ops.append(dict(eng=eng, fn=fn, r=tuple(r), w=tuple(w), dsem=dsem))

    def barrier(self):
        self.bars.append(len(self.ops))

    def mm(self, out, lhsT, rhs, start, stop, r, w):
        self.add('pe', lambda e: e.matmul(out, lhsT, rhs, start=start, stop=stop), r, w)

    def act(self, out, in_, func, r, w, bias=None, scale=None, accum_out=None):
        kw = {}
        if bias is not None: kw['bias'] = bias
        if scale is not None: kw['scale'] = scale
        if accum_out is not None: kw['accum_out'] = accum_out
        self.add('act', lambda e: e.activation(out, in_, func, **kw), r, w)

    def tt(self, eng, out, in0, in1, op, r, w):
        self.add(eng, lambda e: e.tensor_tensor(out, in0, in1, op), r, w)

    def ts(self, eng, out, in0, s1, s2, op0, op1, r, w):
        if op1 is None:
            self.add(eng, lambda e: e.tensor_scalar(out, in0, s1, None, op0), r, w)
        else:
            self.add(eng, lambda e: e.tensor_scalar(out, in0, s1, s2, op0, op1), r, w)

    def stt(self, eng, out, in0, scalar, in1, op0, op1, r, w):
        self.add(eng, lambda e: e.scalar_tensor_tensor(out, in0, scalar, in1, op0, op1), r, w)

    def copy(self, eng, out, in_, r, w):
        if eng == 'act':
            self.add(eng, lambda e: e.copy(out, in_), r, w)
        else:
            self.add(eng, lambda e: e.tensor_copy(out, in_), r, w)

    def recip(self, out, in_, r, w):
        self.add('dve', lambda e: e.reciprocal(out, in_), r, w)

    def memset(self, eng, ap, c, w):
        self.add(eng, lambda e: e.memset(ap, c), (), w)

    def reduce(self, eng, out, in_, op, r, w, axis=None):
        ax = AX.X if axis is None else axis
        self.add(eng, lambda e: e.tensor_reduce(out, in_, ax, op), r, w)

    def dma(self, eng, out, in_, sem, r, w):
        if not sem.startswith('st_'):
            sem = 'ld_' + w[0]
        self.add(eng, lambda e: e.dma_start(out=out, in_=in_), r, w, dsem=sem)

    def analyze(self):
        cnt = {e: 0 for e in ENG}
        dcnt = {}
        tokW, tokR = {}, {}
        waited = {e: {} for e in ENG}
        barneed = {}
        bars = set(self.bars)
        allsig = {}
        for i, op in enumerate(self.ops):
            if i in bars:
                barneed = dict(allsig)
            need = dict(barneed)

            def addneed(d):
                for k, v in d.items():
                    if need.get(k, 0) < v:
                        need[k] = v
            for t in op['r']:
                addneed(tokW.get(t, {}))
            for t in op['w']:
                addneed(tokW.get(t, {}))
                addneed(tokR.get(t, {}))
            if op['dsem']:
                k = ('d', op['dsem'])
                dcnt[k] = dcnt.get(k, 0) + 16
                sig = (k, dcnt[k])
            else:
                k = ('e', op['eng'])
                cnt[op['eng']] += 1
                sig = (k, cnt[op['eng']])
            op['sig'] = sig
            allsig[sig[0]] = sig[1]
            ws = []
            for k, v in need.items():
                if k == ('e', 'pe') and op['eng'] == 'pe' and not op['dsem']:
                    continue
                if waited[op['eng']].get(k, 0) < v:
                    ws.append((k, v))
                    waited[op['eng']][k] = v
            op['waits'] = ws
            for t in op['r']:
                if t in op['w']:
                    continue
                tokR.setdefault(t, {})[sig[0]] = sig[1]
            for t in op['w']:
                if tokR.get(t) or t in op['r']:
                    tokW[t] = {sig[0]: sig[1]}
                    tokR[t] = {}
                else:
                    tokW.setdefault(t, {})[sig[0]] = sig[1]
        self.final = dict(allsig)

    def emit(self):
        self.analyze()
        nc = self.nc
        keys = sorted({op['sig'][0] for op in self.ops})
        for k in keys:
            name = "s_" + "_".join(str(x) for x in k)
            self.sems[k] = self.es.enter_context(nc.semaphore(name))
        with nc.Block() as block:
            def run(name):
                def f(e):
                    for op in self.ops:
                        if op['eng'] != name:
                            continue
                        for k, v in op['waits']:
                            e.wait_ge(self.sems[k], v)
                        ins = op['fn'](e)
                        ins.then_inc(self.sems[op['sig'][0]], 16 if op['dsem'] else 1)
                    if name == 'sp':
                        for k, v in self.final.items():
                            e.wait_ge(self.sems[k], v)
                return f
            block.tensor(run('pe'))
            block.scalar(run('act'))
            block.vector(run('dve'))
            block.gpsimd(run('pool'))
            block.sync(run('sp'))


class Ctx:
    pass


def build(T, stop_after=None, with_moe=True):
    NTOK = NCTX + T
    nc = bass.Bass("TRN2", target_bir_lowering=False)
    din = lambda name, shape, dt=F32: nc.dram_tensor(name, list(shape), dt, kind="ExternalInput").ap()
    dsc = lambda name, shape, dt=F32: nc.dram_tensor(name, list(shape), dt, kind="Internal").ap()
    dout = lambda name, shape, dt=F32: nc.dram_tensor(name, list(shape), dt, kind="ExternalOutput").ap()
    I = Ctx()
    I.xT0 = din("xT0", [D, NTOK])
    I.c2 = din("c2", [128, KT, 2])
    I.ada_w = din("ada_w", [2, D, 6 * D])
    I.ada_b = din("ada_b", [2, 128, 48])
    I.gmix = din("gmix", [2, 128, KT])
    I.gffn = din("gffn", [2, 128, KT])
    I.gfin = din("gfin", [128, KT])
    I.w_in0 = din("w_in0", [D, 4096])
    I.w_out0 = din("w_out0", [D, D])
    I.lblT = din("lblT", [128, 3, 2, 4])
    I.lblrow = din("lblrow", [1, 3 * 2 * 512])
    I.onorm = din("onorm", [128, 1])
    I.subln = din("subln", [128, 1])
    I.dlam = din("dlam", [64, 4])
    I.cosT = din("cosT", [128, NTOK])
    I.sinT = din("sinT", [128, NTOK])
    I.rotm = din("rotm", [128, 128])
    I.consts = din("consts", [128, 6, 128])
    I.router_w = din("router_w", [128, KT, NE])
    I.router_b = din("router_b", [1, NE])
    if with_moe:
        I.w_gate = din("w_gate", [2, NE, D, D])
        I.w_up = din("w_up", [2, NE, D, D])
        I.w_down = din("w_down", [2, NE, D, D])
    I.w_qkv1 = din("w_qkv1", [D, 1536])
    I.w_out1 = din("w_out1", [D, D])
    I.sink = din("sink", [1, 16])
    out = dout("out", [D, T])

    es = ExitStack()
    with es:
        S = Sched(nc, es)
        ARENA = 52000
        arena = es.enter_context(nc.sbuf_tensor("arena", [128, ARENA], F32))
        apos = [0, 0]

        def sb(name, shape, dt=F32):
            free = 1
            for d_ in shape[1:]:
                free *= d_
            n32 = free if dt == F32 else (free + 1) // 2
            n32 = (n32 + 15) // 16 * 16
            off = apos[0]
            apos[0] += n32
            assert apos[0] <= ARENA, (name, apos[0])
            a = arena[0:shape[0], off:off + n32]
            if dt != F32:
                a = a.bitcast(dt)
            a = a[:, 0:free]
            if len(shape) == 3:
                a = a.rearrange("p (a b) -> p a b", b=shape[2])
            elif len(shape) == 4:
                a = a.rearrange("p (a b c) -> p a b c", b=shape[2], c=shape[3])
            elif len(shape) == 6:
                a = a.rearrange("p (a b c d e) -> p a b c d e", b=shape[2], c=shape[3], d=shape[4], e=shape[5])
            return a

        def phase_reset():
            S.barrier()
            apos[0] = apos[1]
        ps = [es.enter_context(nc.psum_tensor(f"ps{i}", [128, 512], F32)) for i in range(8)]
        P = lambda i: f"ps{i}"

        cst = sb("cst", [128, 6, 128])
        cstb = sb("cstb", [128, 6, 128], BF16)
        S.dma('sp', cst[:], I.consts[:, :, :], 'c0', (), ['cst'])
        S.copy('dve', cstb[:], cst[:], ['cst'], ['cstb'])
        IDENT, TLE, TGE, TGT, TLT, ONES = range(6)
        c2s = sb("c2s", [128, KT, 2]); sc2 = sb("sc2", [128, KT, 2])
        adab = sb("adab", [128, 2, 48]); gm = sb("gm", [128, 2, KT]); gf = sb("gf", [128, 2, KT])
        gfin = sb("gfin_s", [128, KT])
        mod = sb("mod", [128, 2, 48, 2])
        S.dma('sp', c2s[:], I.c2[:, :, :], 'c0', (), ['c2s'])
        S.dma('sp', adab[:], I.ada_b.rearrange("l p j -> p l j"), 'c0', (), ['adab'])
        S.dma('sp', gm[:], I.gmix.rearrange("l p j -> p l j"), 'c0', (), ['gm'])
        S.dma('sp', gf[:], I.gffn.rearrange("l p j -> p l j"), 'c0', (), ['gf'])
        S.dma('sp', gfin[:], I.gfin[:, :], 'c0', (), ['gfin'])
        S.act(sc2[:], c2s[:], AF.Silu, ['c2s'], ['sc2'])
        ab = sb("ab", [128, 2, 2, 2, 2, KT])
        LAMBDA_INIT = 0.8 - 0.6 * math.exp(-0.3 * 0)
        dl = sb("dl", [64, 4]); dlp = sb("dlp", [64, 2]); lam = sb("lam", [128, 2]); nlam = sb("nlam", [128, 1])
        subl = sb("subl", [128, 1]); onrm = sb("onrm", [128, 1])
        S.dma('sp', dl[:], I.dlam[:, :], 'x', (), ['dl'])
        S.dma('sp', subl[:], I.subln[:, :], 'x', (), ['subl'])
        S.dma('sp', onrm[:], I.onorm[:, :], 'x', (), ['onrm'])
        S.tt('dve', dlp[:, 0:1], dl[:, 0:1], dl[:, 1:2], ALU.mult, ['dl'], ['dlp'])
        S.tt('dve', dlp[:, 1:2], dl[:, 2:3], dl[:, 3:4], ALU.mult, ['dl'], ['dlp'])
        S.mm(ps[0][:, 0:2], cst[0:64, ONES, :], dlp[:, :], True, True, ['cst', 'dlp'], [P(0)])
        S.act(lam[:], ps[0][:, 0:2], AF.Exp, [P(0)], ['lam'])
        S.tt('dve', nlam[:], lam[:, 1:2], lam[:, 0:1], ALU.subtract, ['lam'], ['nlam'])
        S.ts('dve', nlam[:], nlam[:], -LAMBDA_INIT, None, ALU.add, None, ['nlam'], ['nlam'])
        S.ts('dve', subl[:], subl[:], 1.0 - LAMBDA_INIT, None, ALU.mult, None, ['subl'], ['subl'])

        rot = sb("rot", [128, 128]); rotb = sb("rotb", [128, 128], BF16)
        S.dma('sp', rot[:], I.rotm[:, :], 'c0', (), ['rot'])
        S.copy('dve', rotb[:], rot[:], ['rot'], ['rotb'])
        apos[1] = apos[0]
        wch = [sb(f"wch{i}", [128, KT, 768]) for i in range(2)]
        for l in range(2):
            for ch in range(8):
                buf = wch[ch % 2]; tk = f'wch{ch % 2}'
                S.dma('sp', buf[:], I.ada_w[l, :, ch * 768:(ch + 1) * 768].rearrange("(k p) n -> p k n", p=128),
                      tk, (), [tk])
                for jj in range(6):
                    j = ch * 6 + jj
                    for kt in range(KT):
                        S.mm(ps[0][:, j * 2:j * 2 + 2], buf[:, kt, jj * 128:(jj + 1) * 128], sc2[:, kt, :],
                             kt == 0, kt == KT - 1, [tk, 'sc2'], [P(0)])
            S.tt('dve', mod[:, l, :, :], ps[0][:, 0:96].rearrange("p (j t) -> p j t", t=2),
                 adab[:, l, :].unsqueeze(2).to_broadcast([128, 48, 2]), ALU.add, [P(0), 'adab'], ['mod'])
        for l in range(2):
            for wh, (gg, o) in enumerate(((gm, 0), (gf, 24))):
                for t in range(2):
                    S.stt('dve', ab[:, l, wh, t, 0, :], mod[:, l, o + 8:o + 16, t], 1.0, gg[:, l, :],
                          ALU.add, ALU.mult, ['mod', 'gm', 'gf'], ['ab'])
                    S.copy('dve', ab[:, l, wh, t, 1, :], mod[:, l, o:o + 8, t], ['mod'], ['ab'])

        NB = Ctx()

        def alloc_hn():
            NB.sqb = sb("sqb", [128, 512], BF16); NB.yb = sb("yb", [128, 512], BF16); NB.rr = sb("rr", [128, 512])

        def head_norm_store(o_ap, o_tok, gcol, gtok, dst, qb, extra_mul=None):
            S.act(NB.sqb[:, 0:qb], o_ap, AF.Square, [o_tok], ['sqb'])
            S.mm(ps[0][:, 0:qb], cstb[:, ONES, :], NB.sqb[:, 0:qb], True, True, ['cstb', 'sqb'], [P(0)])
            S.act(NB.rr[:, 0:qb], ps[0][:, 0:qb], AF.Sqrt, [P(0)], ['rr'], bias=EPS, scale=1.0 / 128)
            S.recip(NB.rr[:, 0:qb], NB.rr[:, 0:qb], ['rr'], ['rr'])
            S.stt('dve', NB.rr[:, 0:qb], o_ap, gcol, NB.rr[:, 0:qb], ALU.mult, ALU.mult, [o_tok, 'rr', gtok], ['rr'])
            if extra_mul is not None:
                S.tt('dve', NB.yb[:, 0:qb], NB.rr[:, 0:qb], extra_mul[0], ALU.mult, ['rr', extra_mul[1]], ['yb'])
            else:
                S.copy('dve', NB.yb[:, 0:qb], NB.rr[:, 0:qb], ['rr'], ['yb'])
            S.dma('sp', dst, NB.yb[:, 0:qb], 'st_yb', ['yb'], ['dram'])


        def alloc_norm():
            NB.xt = sb("xt", [128, KT, 512]); NB.sq = sb("sq", [128, KT, 512], BF16)
            NB.rstd = sb("rstd", [128, 512]); NB.hT = sb("hT", [128, KT, 512], BF16)

        def norm_block(src, t0, tb, l, wh, typ, hout=None, htok='hT', f32out=None):
            xt, sq, rstd, hT = NB.xt, NB.sq, NB.rstd, NB.hT
            S.dma('sp', xt[:, :, 0:tb], src[:, t0:t0 + tb].rearrange("(k p) n -> p k n", p=128), 'xt', (), ['xt'])
            S.act(sq[:, :, 0:tb], xt[:, :, 0:tb], AF.Square, ['xt'], ['sq'])
            for kt in range(KT):
                S.mm(ps[7][:, 0:tb], cstb[:, ONES, :], sq[:, kt, 0:tb], kt == 0, kt == KT - 1, ['cstb', 'sq'], [P(7)])
            S.act(rstd[:, 0:tb], ps[7][:, 0:tb], AF.Sqrt, [P(7)], ['rstd'], bias=EPS, scale=1.0 / D)
            S.recip(rstd[:, 0:tb], rstd[:, 0:tb], ['rstd'], ['rstd'])
            ho = hT if hout is None else hout
            for kt in range(KT):
                S.tt('dve', xt[:, kt, 0:tb], xt[:, kt, 0:tb], rstd[:, 0:tb], ALU.mult, ['xt', 'rstd'], ['xt'])
                if f32out is not None:
                    S.ts('dve', xt[:, kt, 0:tb], xt[:, kt, 0:tb], ab[:, l, wh, typ, 0, kt:kt + 1],
                         ab[:, l, wh, typ, 1, kt:kt + 1], ALU.mult, ALU.add, ['xt', 'ab'], ['xt'])
                    S.copy('pool', ho[:, kt, 0:tb], xt[:, kt, 0:tb], ['xt'], [htok])
                else:
                    S.ts('dve', ho[:, kt, 0:tb], xt[:, kt, 0:tb], ab[:, l, wh, typ, 0, kt:kt + 1],
                         ab[:, l, wh, typ, 1, kt:kt + 1], ALU.mult, ALU.add, ['xt', 'ab'], [htok])

        blocks = [(0, NCTX, 1)] + [(NCTX + i * 512, 512, 0) for i in range(T // 512)]
        phase_reset()
        alloc_norm(); hT = NB.hT

        YT = dsc("YT", [D, NTOK], BF16)
        XT1 = dsc("XT1", [D, NTOK], F32)
        hqT = dsc("hqT", [512, NTOK], BF16); hkT = dsc("hkT", [2, 512, NTOK], BF16); hgT = dsc("hgT", [512, NTOK], BF16)
        hlf = dsc("hlf", [2, NTOK, 512], F32); hk = dsc("hk", [2, NTOK, 512], BF16); hv = dsc("hv", [NTOK, 512], BF16)
        dqT = dsc("dqT", [512, NTOK], BF16); dkT = dsc("dkT", [512, NTOK], BF16); dv = dsc("dv", [NTOK, 512], BF16)

        win = sb("win", [128, KT, 4096], BF16)
        for kt in range(KT):
            S.dma('pool', win[:, kt, :], I.w_in0[kt * 128:(kt + 1) * 128, :], 'win', (), ['win'])
        lbT = sb("lbT", [128, 3, 8]); lbs = sb("lbs", [128, 8]); omlT = sb("omlT", [128, 8]); lb0T = sb("lb0T", [128, 8])
        S.dma('sp', lbT[:], I.lblT.rearrange("p l d h -> p l (d h)"), 'c0', (), ['lbT'])
        S.act(lbT[:], lbT[:], AF.Exp, ['lbT'], ['lbT'])
        S.tt('dve', lbs[:], lbT[:, 0, :], lbT[:, 1, :], ALU.add, ['lbT'], ['lbs'])
        S.tt('dve', lbs[:], lbs[:], lbT[:, 2, :], ALU.add, ['lbT', 'lbs'], ['lbs'])
        S.recip(lbs[:], lbs[:], ['lbs'], ['lbs'])
        S.tt('dve', lb0T[:], lbT[:, 0, :], lbs[:], ALU.mult, ['lbT', 'lbs'], ['lb0T'])
        S.ts('dve', omlT[:], lb0T[:], -1.0, 1.0, ALU.mult, ALU.add, ['lb0T'], ['omlT'])
        lbr = sb("lbr", [128, 3, 1024]); lbrs = sb("lbrs", [128, 1024]); lb0r = sb("lb0r", [128, 1024]); omlr = sb("omlr", [128, 1024])
        S.dma('sp', lbr[:], I.lblrow.rearrange("o (l n) -> o l n", l=3).partition_broadcast(128), 'c0', (), ['lbr'])
        S.act(lbr[:], lbr[:], AF.Exp, ['lbr'], ['lbr'])
        S.tt('dve', lbrs[:], lbr[:, 0, :], lbr[:, 1, :], ALU.add, ['lbr'], ['lbrs'])
        S.tt('dve', lbrs[:], lbrs[:], lbr[:, 2, :], ALU.add, ['lbr', 'lbrs'], ['lbrs'])
        S.recip(lbrs[:], lbrs[:], ['lbrs'], ['lbrs'])
        S.tt('dve', lb0r[:], lbr[:, 0, :], lbrs[:], ALU.mult, ['lbr', 'lbrs'], ['lb0r'])
        S.ts('dve', omlr[:], lb0r[:], -1.0, 1.0, ALU.mult, ALU.add, ['lb0r'], ['omlr'])

        cosb = sb("cosb", [128, 512]); sinb = sb("sinb", [128, 512])
        ev = sb("ev", [128, 512]); evb = sb("evb", [128, 512], BF16); ev2 = sb("ev2", [128, 512])
        otb = [sb(f"otb{i}", [128, 512], BF16) for i in range(2)]
        otf = sb("otf", [128, 512])
        nq = [0]

        def store(dst, src_ap, src_tok):
            S.dma('sp', dst, src_ap, 'st_' + src_tok, [src_tok], ['dram'])

        for (t0, tb, typ) in blocks:
            norm_block(I.xT0, t0, tb, 0, 0, typ)
            S.dma('sp', cosb[:, 0:tb], I.cosT[:, t0:t0 + tb], 'cs', (), ['cosb'])
            S.dma('sp', sinb[:, 0:tb], I.sinT[:, t0:t0 + tb], 'cs', (), ['sinb'])
            fm_jobs = []
            for h in range(4): fm_jobs.append(('q', 0 + h * 128, hqT[h * 128:(h + 1) * 128, t0:t0 + tb], h))
            for d in range(2):
                for h in range(4): fm_jobs.append(('k', 512 + d * 512 + h * 128, hkT[d, h * 128:(h + 1) * 128, t0:t0 + tb], d * 4 + h))
            for h in range(4): fm_jobs.append(('g', 2048 + h * 128, hgT[h * 128:(h + 1) * 128, t0:t0 + tb], h))
            for h in range(4): fm_jobs.append(('r', 2560 + h * 128, dqT[h * 128:(h + 1) * 128, t0:t0 + tb], h))
            for h in range(4): fm_jobs.append(('r', 3072 + h * 128, dkT[h * 128:(h + 1) * 128, t0:t0 + tb], h))
            for (kind, c0, dst, idx) in fm_jobs:
                pb = nq[0] % 2; nq[0] += 1
                pp = ps[pb]; ob = otb[pb]; otk = f'otb{pb}'
                for kt in range(KT):
                    S.mm(pp[:, 0:tb], win[:, kt, c0:c0 + 128], hT[:, kt, 0:tb], kt == 0, kt == KT - 1, ['win', 'hT'], [P(pb)])
                if kind in ('q', 'g'):
                    S.act(ob[:, 0:tb], pp[:, 0:tb], AF.Silu, [P(pb)], [otk])
                elif kind == 'k':
                    S.act(ev[:, 0:tb], pp[:, 0:tb], AF.Sigmoid, [P(pb)], ['ev'], scale=-1.0)
                    S.ts('dve', ob[:, 0:tb], ev[:, 0:tb], omlT[:, idx:idx + 1], None, ALU.mult, None, ['ev', 'omlT'], [otk])
                else:
                    S.copy('act', evb[:, 0:tb], pp[:, 0:tb], [P(pb)], ['evb'])
                    S.mm(ps[2][:, 0:tb], rotb[:], evb[:, 0:tb], True, True, ['rotb', 'evb'], [P(2)])
                    S.tt('dve', ev[:, 0:tb], evb[:, 0:tb], cosb[:, 0:tb], ALU.mult, ['evb', 'cosb'], ['ev'])
                    S.tt('dve', ev2[:, 0:tb], ps[2][:, 0:tb], sinb[:, 0:tb], ALU.mult, [P(2), 'sinb'], ['ev2'])
                    S.tt('dve', ob[:, 0:tb], ev[:, 0:tb], ev2[:, 0:tb], ALU.add, ['ev', 'ev2'], [otk])
                store(dst, ob[:, 0:tb], otk)
            for tt_ in range(tb // 128):
                n0 = t0 + tt_ * 128
                for gi, (kind, c0) in enumerate((('f', 512), ('f', 1024), ('c', 1536), ('c', 3584))):
                    pb = 3 + gi % 2; pp = ps[pb]
                    for kt in range(KT):
                        S.mm(pp[:, :], hT[:, kt, tt_ * 128:(tt_ + 1) * 128], win[:, kt, c0:c0 + 512],
                             kt == 0, kt == KT - 1, ['win', 'hT'], [P(pb)])
                    if kind == 'f':
                        d = gi
                        S.act(ev[:, :], pp[:, :], AF.Sigmoid, [P(pb)], ['ev'])
                        S.tt('dve', ev[:, :], ev[:, :], omlr[:, d * 512:(d + 1) * 512], ALU.mult, ['ev', 'omlr'], ['ev'])
                        S.tt('dve', ev[:, :], ev[:, :], lb0r[:, d * 512:(d + 1) * 512], ALU.add, ['ev', 'lb0r'], ['ev'])
                        S.act(otf[:, :], ev[:, :], AF.Ln, ['ev'], ['otf'])
                        store(hlf[d, n0:n0 + 128, :], otf[:, :], 'otf')
                        S.ts('dve', otb[0][:, :], ev[:, :], -1.0, 1.0, ALU.mult, ALU.add, ['ev'], ['otb0'])
                        store(hk[d, n0:n0 + 128, :], otb[0][:, :], 'otb0')
                    else:
                        S.copy('act', otb[1][:, :], pp[:, :], [P(pb)], ['otb1'])
                        store((hv if c0 == 1536 else dv)[n0:n0 + 128, :], otb[1][:, :], 'otb1')
        phase_reset()
        if stop_after == 'A':
            dbg = dout("dbg_hlf", [2, NTOK, 512]); dbg2 = dout("dbg_dkT", [512, NTOK], BF16)
            dbg3 = dout("dbg_hkT", [2, 512, NTOK], BF16)
            S.dma('sp', dbg[:, :, :], hlf[:, :, :], 'dbg', ['dram'], ['dbg'])
            S.dma('sp', dbg2[:, :], dkT[:, :], 'dbg', ['dram'], ['dbg'])
            S.dma('sp', dbg3[:, :, :], hkT[:, :, :], 'dbg', ['dram'], ['dbg'])
            S.emit()
            return nc

        NCH = NTOK // HC
        NCC = NCTX // HC
        LNS = math.log(128 ** -0.5)
        CAP = max(NCC, (NCH + 1) // 2)
        qTh = sb("qTh", [128, NTOK], BF16); vth = sb("vth", [64, CAP, 128], BF16)
        kTh = sb("kTh", [128, CAP * HC], BF16); kth = sb("kth", [64, CAP, 128], BF16)
        lfh = sb("lfh", [64, CAP, 128]); Oacc = sb("Oacc", [128, NTOK]); gTh = sb("gTh", [128, 512], BF16)
        Sst = sb("Sst", [128, 128])
        alloc_hn()
        HB = []
        for i in range(2):
            c_ = Ctx()
            c_.eq = sb(f"eq{i}", [128, 64]); c_.ek = sb(f"ek{i}", [128, 64]); c_.er = sb(f"er{i}", [64, 128])
            c_.QsT = sb(f"QsT{i}", [128, 64], BF16); c_.KsT = sb(f"KsT{i}", [128, 64], BF16)
            c_.Kout = sb(f"Kout{i}", [64, 128], BF16); c_.ATm = sb(f"ATm{i}", [64, 64], BF16)
            c_.Smid = sb(f"Smid{i}", [128, 128], BF16); c_.sc = sb(f"hsc{i}", [128, 4])
            HB.append(c_)
        for h in range(4):
            hs_ = slice(h * 128, (h + 1) * 128)
            S.dma('sp', qTh[:], hqT[hs_, :], 'x', ['dram'], ['qTh'])
            for d in range(2):
                MC, MR = (TLE, TGT) if d == 0 else (TGE, TLT)
                S.memset('dve', Sst[:], 0.0, ['Sst'])
                lohi = [None]
                if d == 0:
                    order = list(range(NCH))
                else:
                    order = list(range(NCC - 1, -1, -1)) + list(range(NCH - 1, NCC - 1, -1))
                for ci, c_abs in enumerate(order):
                    if lohi[0] is None or not (lohi[0][0] <= c_abs < lohi[0][1]):
                        run = [c_abs]
                        for c2 in order[ci + 1:]:
                            lo_, hi_ = min(run + [c2]), max(run + [c2]) + 1
                            if hi_ - lo_ > CAP:
                                break
                            run.append(c2)
                        lo_, hi_ = min(run), max(run) + 1
                        lohi[0] = (lo_, hi_)
                        nch_ = hi_ - lo_
                        S.dma('sp', kTh[:, 0:nch_ * HC], hkT[d, hs_, lo_ * HC:hi_ * HC], 'x', ['dram'], ['kTh'])
                        S.dma('sp', kth[:, 0:nch_, :], hk[d, lo_ * HC:hi_ * HC, :].rearrange("(n p) c -> p n c", p=HC)[:, :, hs_],
                              'x', ['dram'], ['kth'])
                        S.dma('sp', lfh[:, 0:nch_, :], hlf[d, lo_ * HC:hi_ * HC, :].rearrange("(n p) c -> p n c", p=HC)[:, :, hs_],
                              'x', ['dram'], ['lfh'])
                        S.dma('sp', vth[:, 0:nch_, :], hv[lo_ * HC:hi_ * HC, :].rearrange("(n p) c -> p n c", p=HC)[:, :, hs_],
                              'x', ['dram'], ['vth'])
                    c = c_abs - lohi[0][0]
                    B_ = HB[ci % 2]; st = ci % 2
                    pA, pB, pD, pE = ps[4 * st], ps[4 * st + 1], ps[4 * st + 2], ps[4 * st + 3]
                    tA, tB, tD, tE = P(4 * st), P(4 * st + 1), P(4 * st + 2), P(4 * st + 3)
                    sfx = str(st)
                    cs = slice(c_abs * HC, (c_abs + 1) * HC)
                    csl = slice(c * HC, (c + 1) * HC)
                    S.mm(pA[:, 0:64], lfh[:, c, :], cst[0:64, MC, 0:64], True, True, ['lfh', 'cst'], [tA])
                    S.mm(pB[0:64, 0:128], cst[0:64, MR, 0:64], lfh[:, c, :], True, True, ['lfh', 'cst'], [tB])
                    tcol = 63 if d == 0 else 0
                    S.ts('dve', B_.sc[:, 0:1], pA[:, 31:32], -1.0, LNS, ALU.mult, ALU.add, [tA], ['hsc' + sfx])
                    S.copy('dve', B_.sc[:, 1:2], pA[:, 31:32], [tA], ['hsc' + sfx])
                    S.act(B_.sc[:, 2:3], pA[:, tcol:tcol + 1], AF.Exp, [tA], ['hsc' + sfx])
                    S.act(B_.sc[:, 3:4], pA[:, 31:32], AF.Exp, [tA], ['hsc' + sfx])
                    S.act(B_.eq[:], pA[:, 0:64], AF.Exp, [tA, 'hsc' + sfx], ['eq' + sfx], bias=B_.sc[:, 0:1], scale=1.0)
                    S.act(B_.ek[:], pA[:, 0:64], AF.Exp, [tA, 'hsc' + sfx], ['ek' + sfx], bias=B_.sc[:, 1:2], scale=-1.0)
                    S.act(B_.er[:], pB[0:64, 0:128], AF.Exp, [tB], ['er' + sfx])
                    S.tt('dve', B_.QsT[:], qTh[:, cs], B_.eq[:], ALU.mult, ['qTh', 'eq' + sfx], ['QsT' + sfx])
                    S.tt('dve', B_.KsT[:], kTh[:, csl], B_.ek[:], ALU.mult, ['kTh', 'ek' + sfx], ['KsT' + sfx])
                    S.tt('dve', B_.Kout[:], kth[:, c, :], B_.er[:], ALU.mult, ['kth', 'er' + sfx], ['Kout' + sfx])
                    S.mm(pB[0:64, 128:192], B_.KsT[:], B_.QsT[:], True, True, ['KsT' + sfx, 'QsT' + sfx], [tB])
                    S.tt('dve', B_.ATm[:], pB[0:64, 128:192], cst[0:64, MC, 0:64], ALU.mult, [tB, 'cst'], ['ATm' + sfx])
                    S.ts('dve', B_.Smid[:], Sst[:], B_.sc[:, 3:4], None, ALU.mult, None, ['Sst', 'hsc' + sfx], ['Smid' + sfx])
                    S.mm(pD[:, 0:64], vth[:, c, :], B_.ATm[:], True, False, ['vth', 'ATm' + sfx], [tD])
                    S.mm(pD[:, 0:64], B_.Smid[:], B_.QsT[:], False, True, ['Smid' + sfx, 'QsT' + sfx], [tD])
                    if d == 0:
                        S.copy('act', Oacc[:, cs], pD[:, 0:64], [tD], ['Oacc'])
                    else:
                        S.tt('dve', Oacc[:, cs], pD[:, 0:64], Oacc[:, cs], ALU.add, [tD, 'Oacc'], ['Oacc'])
                    S.mm(pE[:, 0:128], B_.Kout[:], vth[:, c, :], True, True, ['Kout' + sfx, 'vth'], [tE])
                    S.stt('dve', Sst[:], Sst[:], B_.sc[:, 2:3], pE[:, 0:128], ALU.mult, ALU.add,
                          ['Sst', 'hsc' + sfx, tE], ['Sst'])
            for (t0, tb, typ) in blocks:
                S.dma('sp', gTh[:, 0:tb], hgT[hs_, t0:t0 + tb], 'x', ['dram'], ['gTh'])
                head_norm_store(Oacc[:, t0:t0 + tb], 'Oacc', onrm[:, 0:1], 'onrm', YT[hs_, t0:t0 + tb], tb,
                                extra_mul=(gTh[:, 0:tb], 'gTh'))
        phase_reset()
        if stop_after == 'B':
            dbg = dout("dbg_YT", [D, NTOK], BF16)
            S.dma('sp', dbg[:, :], YT[:, :], 'x', ['dram'], ['dbg'])
            S.emit()
            return nc

        NKT = NTOK // 128
        KTh = sb("KTh", [128, NTOK], BF16); Vh = sb("Vh", [128, NKT, 128], BF16)
        QTh = sb("QTh", [128, 512], BF16)
        e1 = [sb(f"e1_{i}", [128, 512], BF16) for i in range(2)]
        e2 = [sb(f"e2_{i}", [128, 512], BF16) for i in range(2)]
        oa = sb("oa", [128, 512]); ob2 = sb("ob2", [128, 512])
        alloc_hn(); rr = NB.rr

        for h in range(4):
            S.dma('sp', KTh[:], dkT[h * 128:(h + 1) * 128, :], 'x', ['dram'], ['KTh'])
            S.dma('sp', Vh[:], dv.rearrange("(n p) c -> p n c", p=128)[:, :, h * 128:(h + 1) * 128], 'x', ['dram'], ['Vh'])
            qblocks = [(0, NCTX, 0, NCTX // 128)] + [(NCTX + i * 512, 512, 0, NKT) for i in range(T // 512)]
            for (q0, qb, k0, k1) in qblocks:
                S.dma('sp', QTh[:, 0:qb], dqT[h * 128:(h + 1) * 128, q0:q0 + qb], 'x', ['dram'], ['QTh'])
                for ki, kt_ in enumerate(range(k0, k1)):
                    a = ki % 2
                    sA, sB = (0, 1) if a == 0 else (6, 7)
                    ks = slice(kt_ * 128, (kt_ + 1) * 128)
                    S.mm(ps[sA][:, 0:qb], KTh[0:64, ks], QTh[0:64, 0:qb], True, True, ['KTh', 'QTh'], [P(sA)])
                    S.mm(ps[sB][:, 0:qb], KTh[64:128, ks], QTh[64:128, 0:qb], True, True, ['KTh', 'QTh'], [P(sB)])
                    S.act(e1[a][:, 0:qb], ps[sA][:, 0:qb], AF.Exp, [P(sA)], [f'e1_{a}'], scale=0.125)
                    S.act(e2[a][:, 0:qb], ps[sB][:, 0:qb], AF.Exp, [P(sB)], [f'e2_{a}'], scale=0.125)
                    first, last = (kt_ == k0), (kt_ == k1 - 1)
                    S.mm(ps[2][:, 0:qb], Vh[:, kt_, :], e1[a][:, 0:qb], first, last, ['Vh', f'e1_{a}'], [P(2)])
                    S.mm(ps[3][:, 0:qb], Vh[:, kt_, :], e2[a][:, 0:qb], first, last, ['Vh', f'e2_{a}'], [P(3)])
                    S.mm(ps[4][:, 0:qb], cstb[:, ONES, :], e1[a][:, 0:qb], first, last, ['cstb', f'e1_{a}'], [P(4)])
                    S.mm(ps[5][:, 0:qb], cstb[:, ONES, :], e2[a][:, 0:qb], first, last, ['cstb', f'e2_{a}'], [P(5)])
                S.recip(rr[:, 0:qb], ps[4][:, 0:qb], [P(4)], ['rr'])
                S.tt('dve', oa[:, 0:qb], ps[2][:, 0:qb], rr[:, 0:qb], ALU.mult, [P(2), 'rr'], ['oa'])
                S.recip(rr[:, 0:qb], ps[5][:, 0:qb], [P(5)], ['rr'])
                S.tt('dve', ob2[:, 0:qb], ps[3][:, 0:qb], rr[:, 0:qb], ALU.mult, [P(3), 'rr'], ['ob2'])
                S.stt('dve', oa[:, 0:qb], ob2[:, 0:qb], nlam[:, 0:1], oa[:, 0:qb], ALU.mult, ALU.add, ['ob2', 'nlam', 'oa'], ['oa'])
                head_norm_store(oa[:, 0:qb], 'oa', subl[:, 0:1], 'subl', YT[512 + h * 128:512 + (h + 1) * 128, q0:q0 + qb], qb)
        phase_reset()
        if stop_after == 'C':
            dbg = dout("dbg_YT", [D, NTOK], BF16)
            S.dma('sp', dbg[:, :], YT[:, :], 'x', ['dram'], ['dbg'])
            S.emit()
            return nc

        XT2 = dsc("XT2", [D, NTOK], F32)
        XT3 = dsc("XT3", [D, NTOK], F32)
        XT4 = dsc("XT4", [D, NTOK], F32)
        WRT = dsc("WRT", [NE, NTOK], F32)

        def outproj_phase(l, w_dram, xsrc, xdst, blks):
            wo = sb("wo", [128, KT, D], BF16)
            for kt in range(KT):
                S.dma('pool', wo[:, kt, :], w_dram[kt * 128:(kt + 1) * 128, :], 'x', (), ['wo'])
            Yb = sb("Yb", [128, KT, 512], BF16); xo = sb("xo", [128, KT, 512])
            for (t0, tb, typ) in blks:
                S.dma('sp', Yb[:, :, 0:tb], YT[:, t0:t0 + tb].rearrange("(k p) n -> p k n", p=128), 'x', ['dram'], ['Yb'])
                S.dma('sp', xo[:, :, 0:tb], xsrc[:, t0:t0 + tb].rearrange("(k p) n -> p k n", p=128), 'x', ['dram'], ['xo'])
                for dt_ in range(KT):
                    pb = dt_ % 2
                    for kt in range(KT):
                        S.mm(ps[pb][:, 0:tb], wo[:, kt, dt_ * 128:(dt_ + 1) * 128], Yb[:, kt, 0:tb],
                             kt == 0, kt == KT - 1, ['wo', 'Yb'], [P(pb)])
                    S.stt('dve', xo[:, dt_, 0:tb], ps[pb][:, 0:tb], mod[:, l, 16 + dt_, typ:typ + 1], xo[:, dt_, 0:tb],
                          ALU.mult, ALU.add, [P(pb), 'mod', 'xo'], ['xo'])
                S.dma('sp', xdst[:, t0:t0 + tb].rearrange("(k p) n -> p k n", p=128), xo[:, :, 0:tb], 'st_xo', ['xo'], ['dram'])
            phase_reset()

        outproj_phase(0, I.w_out0, I.xT0, XT1, blocks)
        if stop_after == 'D':
            dbg = dout("dbg_X", [D, NTOK], F32)
            S.dma('sp', dbg[:, :], XT1[:, :], 'x', ['dram'], ['dbg'])
            S.emit()
            return nc

        def moe_phase(l, xsrc, xdst, blks):
            NB.xt = sb("xt", [128, KT, 512]); NB.sq = sb("sq", [128, KT, 512], BF16)
            NB.rstd = sb("rstd", [128, 512]); NB.hT = None
            SBMAX = 1024
            rw = sb("rw", [128, KT, NE]); rb = sb("rb", [128, NE])
            S.dma('sp', rw[:], I.router_w[:, :, :], 'x', (), ['rw'])
            S.dma('sp', rb[:], I.router_b.partition_broadcast(128), 'x', (), ['rb'])
            h2 = sb("h2", [128, KT, SBMAX], BF16); xacc = sb("xacc", [128, KT, SBMAX])
            Re = sb("Re", [128, SBMAX]); WT = sb("WT", [NE, 512])
            wbuf = {}
            for nm in ('g', 'u', 'd'):
                for i in range(2):
                    wbuf[nm, i] = sb(f"w{nm}{i}", [128, KT, D], BF16)
            hid = sb("hid", [128, KT, 512], BF16); sg = sb("sg", [128, 512]); tmp = sb("tmpm", [128, 512])
            r_aff = sb("r_aff", [128, NE]); r_sel = sb("r_sel", [128, NE]); r_eq = sb("r_eq", [128, NE])
            r_s2 = sb("r_s2", [128, NE]); r_m = sb("r_m", [128, 12]); r_w = sb("r_w", [128, NE])
            g3 = lambda a: a[:, :].rearrange("p (g e) -> p g e", e=4)
            sbs = []; cur = []; n = 0
            for b_ in blks:
                if n + b_[1] > SBMAX:
                    sbs.append(cur); cur = []; n = 0
                cur.append(b_); n += b_[1]
            sbs.append(cur)
            ecount = [0]
            for sblk in sbs:
                n0 = sblk[0][0]; ntot = sum(b_[1] for b_ in sblk)
                off = 0
                for (t0, tb, typ) in sblk:
                    norm_block(xsrc, t0, tb, l, 1, typ, hout=h2[:, :, off:off + tb], htok='h2', f32out=True)
                    S.dma('sp', xacc[:, :, off:off + tb], xsrc[:, t0:t0 + tb].rearrange("(k p) n -> p k n", p=128),
                          'x', ['dram'], ['xacc'])
                    xt = NB.xt
                    for tt_ in range(tb // 128):
                        ts_ = slice(tt_ * 128, (tt_ + 1) * 128)
                        for kt in range(KT):
                            S.mm(ps[6][:, 0:NE], xt[:, kt, ts_], rw[:, kt, :], kt == 0, kt == KT - 1, ['xt', 'rw'], [P(6)])
                        S.act(r_aff[:], ps[6][:, 0:NE], AF.Sigmoid, [P(6)], ['r_aff'])
                        S.tt('dve', r_sel[:], r_aff[:], rb[:], ALU.add, ['r_aff', 'rb'], ['r_sel'])
                        S.reduce('dve', r_m[:, 0:4], g3(r_sel), ALU.max, ['r_sel'], ['r_m'])
                        S.tt('dve', g3(r_eq), g3(r_sel), r_m[:, 0:4].unsqueeze(2).to_broadcast([128, 4, 4]), ALU.is_equal,
                             ['r_sel', 'r_m'], ['r_eq'])
                        S.stt('dve', r_s2[:], r_eq[:], -1.0e9, r_sel[:], ALU.mult, ALU.add, ['r_eq', 'r_sel'], ['r_s2'])
                        S.reduce('dve', r_m[:, 4:8], g3(r_s2), ALU.max, ['r_s2'], ['r_m'])
                        S.tt('dve', r_m[:, 8:12], r_m[:, 0:4], r_m[:, 4:8], ALU.add, ['r_m'], ['r_m'])
                        S.reduce('dve', r_m[:, 0:1], r_m[:, 8:12], ALU.max, ['r_m'], ['r_m'])
                        S.ts('dve', r_m[:, 8:12], r_m[:, 8:12], r_m[:, 0:1], None, ALU.is_equal, None, ['r_m'], ['r_m'])
                        S.tt('dve', g3(r_eq), g3(r_sel), r_m[:, 4:8].unsqueeze(2).to_broadcast([128, 4, 4]), ALU.is_ge,
                             ['r_sel', 'r_m'], ['r_eq'])
                        S.tt('dve', g3(r_eq), g3(r_eq), r_m[:, 8:12].unsqueeze(2).to_broadcast([128, 4, 4]), ALU.mult,
                             ['r_eq', 'r_m'], ['r_eq'])
                        S.tt('dve', r_w[:], r_aff[:], r_eq[:], ALU.mult, ['r_aff', 'r_eq'], ['r_w'])
                        S.reduce('dve', r_m[:, 1:2], r_w[:], ALU.add, ['r_w'], ['r_m'])
                        S.recip(r_m[:, 1:2], r_m[:, 1:2], ['r_m'], ['r_m'])
                        S.ts('dve', r_w[:], r_w[:], r_m[:, 1:2], None, ALU.mult, None, ['r_w', 'r_m'], ['r_w'])
                        S.mm(ps[7][0:NE, 0:128], r_w[:], cst[:, IDENT, :], True, True, ['r_w', 'cst'], [P(7)])
                        S.copy('act', WT[:, ts_], ps[7][0:NE, 0:128], [P(7)], ['WT'])
                    S.dma('sp', WRT[:, t0:t0 + tb], WT[:, 0:tb], 'st_WT', ['WT'], ['dramW'])
                    off += tb
                for e in range(NE):
                    bi = ecount[0] % 2; ecount[0] += 1
                    wg, wu, wd = wbuf['g', bi], wbuf['u', bi], wbuf['d', bi]
                    tg, tu, td = f'wg{bi}', f'wu{bi}', f'wd{bi}'
                    for kt in range(KT):
                        S.dma('pool', wg[:, kt, :], I.w_gate[l, e, kt * 128:(kt + 1) * 128, :], 'x', (), [tg])
                        S.dma('pool', wu[:, kt, :], I.w_up[l, e, kt * 128:(kt + 1) * 128, :], 'x', (), [tu])
                        S.dma('pool', wd[:, kt, :], I.w_down[l, e, kt * 128:(kt + 1) * 128, :], 'x', (), [td])
                    S.dma('sp', Re[:, 0:ntot], WRT[e:e + 1, n0:n0 + ntot].partition_broadcast(128), 'x', ['dramW'], ['Re'])
                    off = 0
                    for (t0, tb, typ) in sblk:
                        for nt in range(KT):
                            a = nt % 2
                            for kt in range(KT):
                                S.mm(ps[a][:, 0:tb], wg[:, kt, nt * 128:(nt + 1) * 128], h2[:, kt, off:off + tb],
                                     kt == 0, kt == KT - 1, [tg, 'h2'], [P(a)])
                            for kt in range(KT):
                                S.mm(ps[2 + a][:, 0:tb], wu[:, kt, nt * 128:(nt + 1) * 128], h2[:, kt, off:off + tb],
                                     kt == 0, kt == KT - 1, [tu, 'h2'], [P(2 + a)])
                            S.act(sg[:, 0:tb], ps[a][:, 0:tb], AF.Silu, [P(a)], ['sg'])
                            S.tt('dve', hid[:, nt, 0:tb], ps[2 + a][:, 0:tb], sg[:, 0:tb], ALU.mult, [P(2 + a), 'sg'], ['hid'])
                        for dt_ in range(KT):
                            a = 4 + dt_ % 2
                            for nt in range(KT):
                                S.mm(ps[a][:, 0:tb], wd[:, nt, dt_ * 128:(dt_ + 1) * 128], hid[:, nt, 0:tb],
                                     nt == 0, nt == KT - 1, [td, 'hid'], [P(a)])
                            S.stt('dve', tmp[:, 0:tb], ps[a][:, 0:tb], mod[:, l, 40 + dt_, typ:typ + 1], Re[:, off:off + tb],
                                  ALU.mult, ALU.mult, [P(a), 'mod', 'Re'], ['tmpm'])
                            S.tt('pool', xacc[:, dt_, off:off + tb], xacc[:, dt_, off:off + tb], tmp[:, 0:tb], ALU.add,
                                 ['xacc', 'tmpm'], ['xacc'])
                        off += tb
                S.dma('sp', xdst[:, n0:n0 + ntot].rearrange("(k p) n -> p k n", p=128), xacc[:, :, 0:ntot],
                      'st_xacc', ['xacc'], ['dram'])
            phase_reset()

        if with_moe:
            moe_phase(0, XT1, XT2, blocks)
        else:
            XT2 = XT1
        if stop_after == 'E':
            dbg = dout("dbg_X", [D, NTOK], F32)
            S.dma('sp', dbg[:, :], XT2[:, :], 'x', ['dram'], ['dbg'])
            S.emit()
            return nc

        latblocks = blocks[1:]
        qT1 = dsc("qT1", [D, NTOK], BF16); kT1 = dsc("kT1", [256, NTOK], BF16); v1 = dsc("v1", [NTOK, 256], BF16)
        alloc_norm(); hT = NB.hT
        wq = sb("wq", [128, KT, 1536], BF16)
        for kt in range(KT):
            S.dma('pool', wq[:, kt, :], I.w_qkv1[kt * 128:(kt + 1) * 128, :], 'x', (), ['wq'])
        cosb = sb("cosb1", [128, 512]); sinb = sb("sinb1", [128, 512])
        ev = sb("ev1", [128, 512]); evb = sb("evb1", [128, 512], BF16); ev2 = sb("ev21", [128, 512])
        otb = [sb(f"otb1_{i}", [128, 512], BF16) for i in range(2)]
        for (t0, tb, typ) in blocks:
            norm_block(XT2, t0, tb, 1, 0, typ)
            S.dma('sp', cosb[:, 0:tb], I.cosT[:, t0:t0 + tb], 'x', (), ['cosb1'])
            S.dma('sp', sinb[:, 0:tb], I.sinT[:, t0:t0 + tb], 'x', (), ['sinb1'])
            for ct in range(10):
                pb = ct % 2; pp = ps[pb]; ob = otb[pb]; otk = f'otb1_{pb}'
                for kt in range(KT):
                    S.mm(pp[:, 0:tb], wq[:, kt, ct * 128:(ct + 1) * 128], hT[:, kt, 0:tb], kt == 0, kt == KT - 1, ['wq', 'hT'], [P(pb)])
                S.copy('act', evb[:, 0:tb], pp[:, 0:tb], [P(pb)], ['evb1'])
                S.mm(ps[2][:, 0:tb], rotb[:], evb[:, 0:tb], True, True, ['rotb', 'evb1'], [P(2)])
                S.tt('dve', ev[:, 0:tb], evb[:, 0:tb], cosb[:, 0:tb], ALU.mult, ['evb1', 'cosb1'], ['ev1'])
                S.tt('dve', ev2[:, 0:tb], ps[2][:, 0:tb], sinb[:, 0:tb], ALU.mult, [P(2), 'sinb1'], ['ev21'])
                S.tt('dve', ob[:, 0:tb], ev[:, 0:tb], ev2[:, 0:tb], ALU.add, ['ev1', 'ev21'], [otk])
                dst = qT1[ct * 128:(ct + 1) * 128, t0:t0 + tb] if ct < 8 else kT1[(ct - 8) * 128:(ct - 7) * 128, t0:t0 + tb]
                S.dma('sp', dst, ob[:, 0:tb], 'st_' + otk, [otk], ['dram'])
            for tt_ in range(tb // 128):
                n0 = t0 + tt_ * 128
                for kt in range(KT):
                    S.mm(ps[3][:, 0:256], hT[:, kt, tt_ * 128:(tt_ + 1) * 128], wq[:, kt, 1280:1536], kt == 0, kt == KT - 1,
                         ['wq', 'hT'], [P(3)])
                S.copy('act', otb[0][:, 0:256], ps[3][:, 0:256], [P(3)], ['otb1_0'])
                S.dma('sp', v1[n0:n0 + 128, :], otb[0][:, 0:256], 'st_otb1_0', ['otb1_0'], ['dram'])
        phase_reset()

        NKT = NTOK // 128
        NQB = T // 128
        kT4 = sb("kT4", [64, NTOK], BF16); q4 = sb("q4", [64, 4, NTOK], BF16); v4 = sb("v4", [128, NKT, 64], BF16)
        esk = sb("esk", [64, 16])
        S.dma('sp', esk[:], I.sink.partition_broadcast(64), 'x', (), ['esk'])
        S.act(esk[:], esk[:], AF.Exp, ['esk'], ['esk'])
        eb = [sb(f"eb{i}", [128, 4, 128], BF16) for i in range(2)]
        ef = [sb(f"ef{i}", [128, 4, 128]) for i in range(2)]
        den = sb("den", [64, 4, 128]); o4 = sb("o4", [64, 4, 128], BF16)
        for kvh in range(4):
            S.dma('sp', kT4[:], kT1[kvh * 64:(kvh + 1) * 64, :], 'x', ['dram'], ['kT4'])
            S.dma('sp', q4[:], qT1[kvh * 256:(kvh + 1) * 256, :].rearrange("(g d) n -> d g n", d=64), 'x', ['dram'], ['q4'])
            S.dma('sp', v4[:], v1.rearrange("(n p) c -> p n c", p=128)[:, :, kvh * 64:(kvh + 1) * 64], 'x', ['dram'], ['v4'])
            for n in range(NQB):
                qc = slice(NCTX + n * 128, NCTX + (n + 1) * 128)
                tiles = [(0, None), (1, None)]
                if n > 0: tiles.append((2 + n - 1, TGE))
                tiles.append((2 + n, None))
                if n < NQB - 1: tiles.append((2 + n + 1, TLE))
                pso, psd = ps[4 + (n % 2) * 2], ps[5 + (n % 2) * 2]
                to, td = P(4 + (n % 2) * 2), P(5 + (n % 2) * 2)
                for ki, (kt_, msk) in enumerate(tiles):
                    a = ki % 2
                    S.mm(ps[a][:, :], kT4[:, kt_ * 128:(kt_ + 1) * 128], q4[:, :, qc], True, True, ['kT4', 'q4'], [P(a)])
                    first, last = ki == 0, ki == len(tiles) - 1
                    if msk is None:
                        S.act(eb[a][:], ps[a][:, :].rearrange("p (g n) -> p g n", g=4), AF.Exp, [P(a)], [f'eb{a}'], scale=0.125)
                    else:
                        S.act(ef[a][:], ps[a][:, :].rearrange("p (g n) -> p g n", g=4), AF.Exp, [P(a)], [f'ef{a}'], scale=0.125)
                        S.tt('dve', eb[a][:], ef[a][:], cst[:, msk, :].unsqueeze(1).to_broadcast([128, 4, 128]), ALU.mult,
                             [f'ef{a}', 'cst'], [f'eb{a}'])
                    S.mm(pso[0:64, :], v4[:, kt_, :], eb[a][:], first, last, ['v4', f'eb{a}'], [to])
                    S.mm(psd[0:64, :], cstb[:, ONES, 0:64], eb[a][:], first, last, ['cstb', f'eb{a}'], [td])
                for g in range(4):
                    hq = kvh * 4 + g
                    S.ts('dve', den[:, g, :], psd[0:64, g * 128:(g + 1) * 128], esk[:, hq:hq + 1], None, ALU.add, None,
                         [td, 'esk'], ['den'])
                S.recip(den[:], den[:], ['den'], ['den'])
                S.tt('dve', o4[:], pso[0:64, :].rearrange("p (g n) -> p g n", g=4), den[:], ALU.mult, [to, 'den'], ['o4'])
                S.dma('sp', YT[kvh * 256:(kvh + 1) * 256, qc].rearrange("(g d) n -> d g n", d=64), o4[:], 'st_o4', ['o4'], ['dram'])
        phase_reset()
        if stop_after == 'G':
            dbg = dout("dbg_YT", [D, NTOK], BF16)
            S.dma('sp', dbg[:, :], YT[:, :], 'x', ['dram'], ['dbg'])
            S.emit()
            return nc

        outproj_phase(1, I.w_out1, XT2, XT3, latblocks)
        if with_moe:
            moe_phase(1, XT3, XT4, latblocks)
        else:
            XT4 = XT3

        alloc_norm()
        xt, sq, rstd = NB.xt, NB.sq, NB.rstd
        for (t0, tb, typ) in latblocks:
            S.dma('sp', xt[:, :, 0:tb], XT4[:, t0:t0 + tb].rearrange("(k p) n -> p k n", p=128), 'x', ['dram'], ['xt'])
            S.act(sq[:, :, 0:tb], xt[:, :, 0:tb], AF.Square, ['xt'], ['sq'])
            for kt in range(KT):
                S.mm(ps[7][:, 0:tb], cstb[:, ONES, :], sq[:, kt, 0:tb], kt == 0, kt == KT - 1, ['cstb', 'sq'], [P(7)])
            S.act(rstd[:, 0:tb], ps[7][:, 0:tb], AF.Sqrt, [P(7)], ['rstd'], bias=EPS, scale=1.0 / D)
            S.recip(rstd[:, 0:tb], rstd[:, 0:tb], ['rstd'], ['rstd'])
            for kt in range(KT):
                S.stt('dve', xt[:, kt, 0:tb], xt[:, kt, 0:tb], gfin[:, kt:kt + 1], rstd[:, 0:tb], ALU.mult, ALU.mult,
                      ['xt', 'rstd', 'gfin'], ['xt'])
            S.dma('sp', out[:, t0 - NCTX:t0 - NCTX + tb].rearrange("(k p) n -> p k n", p=128), xt[:, :, 0:tb], 'st_xt', ['xt'], ['out'])
        S.emit()
    return nc


def rope_tables(T):
    gw = 64
    n_rows = T // gw
    row = np.repeat(np.arange(n_rows), gw).astype(np.float32)
    col = np.tile(np.arange(gw), n_rows).astype(np.float32)
    half = 32
    inv = (1.0 / (np.float32(10000.0) ** (np.arange(0, half, 2, dtype=np.float32) / np.float32(half)))).astype(np.float32)
    ar = row[:, None] * inv
    ac = col[:, None] * inv
    ang = np.concatenate([ar, ar, ac, ac], axis=-1)
    return np.cos(ang).astype(np.float32), np.sin(ang).astype(np.float32)


def fm(v):
    return np.ascontiguousarray(v.reshape(KT, 128).T)


def prep_inputs(inp, b, T, with_moe=True):
    NTOK = NCTX + T
    m = {}
    m["xT0"] = np.ascontiguousarray(np.concatenate([inp["ctx"][b], inp["x"][b]], axis=0).T)
    cc = np.stack([inp["c"][b], inp["c_ctx"]], axis=-1)
    m["c2"] = np.ascontiguousarray(cc.reshape(KT, 128, 2).transpose(1, 0, 2))
    m["ada_w"] = inp["ada_w"]
    m["ada_b"] = np.ascontiguousarray(inp["ada_b"].reshape(2, 48, 128).transpose(0, 2, 1))
    m["gmix"] = np.ascontiguousarray(inp["norm_mix_g"].reshape(2, KT, 128).transpose(0, 2, 1))
    m["gffn"] = np.ascontiguousarray(inp["norm_ffn_g"].reshape(2, KT, 128).transpose(0, 2, 1))
    m["gfin"] = fm(inp["final_norm_g"])
    m["w_in0"] = inp["even_w_in"][0]
    m["w_out0"] = inp["even_w_out"][0]
    lg = inp["hgrn_lb_logits"]
    m["lblT"] = np.ascontiguousarray(lg.reshape(3, 2, 4, 128).transpose(3, 0, 1, 2))
    m["lblrow"] = np.ascontiguousarray(lg.reshape(1, -1))
    m["onorm"] = np.ascontiguousarray(inp["hgrn_onorm_g"][0].reshape(128, 1))
    m["subln"] = np.ascontiguousarray(inp["diff_subln_g"][0].reshape(128, 1))
    m["dlam"] = np.ascontiguousarray(inp["diff_lambda"][0].T)
    m["router_w"] = np.ascontiguousarray(inp["router_w"].reshape(KT, 128, NE).transpose(1, 0, 2))
    m["router_b"] = np.ascontiguousarray(inp["router_b"].reshape(1, NE))
    if with_moe:
        m["w_gate"] = inp["moe_w_gate"]; m["w_up"] = inp["moe_w_up"]; m["w_down"] = inp["moe_w_down"]
    m["w_qkv1"] = inp["odd_w_qkv"][0]; m["w_out1"] = inp["odd_w_out"][0]
    m["sink"] = np.ascontiguousarray(inp["swa_sink"][0].reshape(1, 16))
    cos, sin = rope_tables(T)
    cosT = np.ones((128, NTOK), np.float32); sinT = np.zeros((128, NTOK), np.float32)
    cosT[0:64, NCTX:] = cos.T; cosT[64:128, NCTX:] = cos.T
    sinT[0:64, NCTX:] = sin.T; sinT[64:128, NCTX:] = sin.T
    m["cosT"] = cosT; m["sinT"] = sinT
    R = np.zeros((128, 128), np.float32)
    for o in (0, 64):
        for i in range(16):
            R[o + i, o + 16 + i] = -1.0; R[o + 16 + i, o + i] = 1.0
            R[o + 32 + i, o + 48 + i] = -1.0; R[o + 48 + i, o + 32 + i] = 1.0
    m["rotm"] = np.ascontiguousarray(R.T)
    s_ = np.arange(128)[:, None]; t_ = np.arange(128)[None, :]
    cs = np.stack([np.eye(128), s_ <= t_, s_ >= t_, s_ > t_, s_ < t_, np.ones((128, 128))], axis=1).astype(np.float32)
    m["consts"] = np.ascontiguousarray(cs)
    return m


_T_FULL = 8192


def kernel(**inputs):
    inp = {k: np.asarray(v) for k, v in inputs.items()}
    T = inp["x"].shape[1]
    nc = build(T)
    maps = [prep_inputs(inp, b, T) for b in range(2)]
    in_maps = [maps[c % 2] for c in range(8)]
    res = run_bass_kernel_spmd(nc, in_maps, core_ids=list(range(8)))
    outs = [np.ascontiguousarray(np.asarray(res.results[b]["out"]).T) for b in range(2)]
    return np.stack(outs, axis=0).astype(np.float32)
```

```python
import math
import numpy as np
import concourse.bass as bass
import concourse.mybir as mybir
from concourse.bass_utils import run_bass_kernel_spmd
from contextlib import ExitStack

F32 = mybir.dt.float32
BF16 = mybir.dt.bfloat16
AF = mybir.ActivationFunctionType
ALU = mybir.AluOpType
AX = mybir.AxisListType
ENG = ['pe', 'act', 'dve', 'pool', 'sp']

D = 1024
KT = 8
NCTX = 256
EPS = 1e-6
NE = 16
HC = 64


class Sched:
    def __init__(self, nc, es):
        self.nc = nc
        self.es = es
        self.ops = []
        self.sems = {}
        self.bars = []

    def add(self, eng, fn, r=(), w=(), dsem=None):
        self.ops.append(dict(eng=eng, fn=fn, r=tuple(r), w=tuple(w), dsem=dsem))

    def barrier(self):
        self.bars.append(len(self.ops))

    def mm(self, out, lhsT, rhs, start, stop, r, w):
        self.add('pe', lambda e: e.matmul(out, lhsT, rhs, start=start, stop=stop), r, w)

    def act(self, out, in_, func, r, w, bias=None, scale=None, accum_out=None):
        kw = {}
        if bias is not None: kw['bias'] = bias
        if scale is not None: kw['scale'] = scale
        if accum_out is not None: kw['accum_out'] = accum_out
        self.add('act', lambda e: e.activation(out, in_, func, **kw), r, w)

    def tt(self, eng, out, in0, in1, op, r, w):
        self.add(eng, lambda e: e.tensor_tensor(out, in0, in1, op), r, w)

    def ts(self, eng, out, in0, s1, s2, op0, op1, r, w):
        if op1 is None:
            self.add(eng, lambda e: e.tensor_scalar(out, in0, s1, None, op0), r, w)
        else:
            self.add(eng, lambda e: e.tensor_scalar(out, in0, s1, s2, op0, op1), r, w)

    def stt(self, eng, out, in0, scalar, in1, op0, op1, r, w):
        self.add(eng, lambda e: e.scalar_tensor_tensor(out, in0, scalar, in1, op0, op1), r, w)

    def copy(self, eng, out, in_, r, w):
        if eng == 'act':
            self.add(eng, lambda e: e.copy(out, in_), r, w)
        else:
            self.add(eng, lambda e: e.tensor_copy(out, in_), r, w)

    def recip(self, out, in_, r, w):
        self.add('dve', lambda e: e.reciprocal(out, in_), r, w)

    def memset(self, eng, ap, c, w):
        self.add(eng, lambda e: e.memset(ap, c), (), w)

    def reduce(self, eng, out, in_, op, r, w, axis=None):
        ax = AX.X if axis is None else axis
        self.add(eng, lambda e: e.tensor_reduce(out, in_, ax, op), r, w)

    def dma(self, eng, out, in_, sem, r, w):
        if not sem.startswith('st_'):
            sem = 'ld_' + w[0]
        self.add(eng, lambda e: e.dma_start(out=out, in_=in_), r, w, dsem=sem)

    def analyze(self):
        cnt = {e: 0 for e in ENG}
        dcnt = {}
        tokW, tokR = {}, {}
        waited = {e: {} for e in ENG}
        barneed = {}
        bars = set(self.bars)
        allsig = {}
        for i, op in enumerate(self.ops):
            if i in bars:
                barneed = dict(allsig)
            need = dict(barneed)

            def addneed(d):
                for k, v in d.items():
                    if need.get(k, 0) < v:
                        need[k] = v
            for t in op['r']:
                addneed(tokW.get(t, {}))
            for t in op['w']:
                addneed(tokW.get(t, {}))
                addneed(tokR.get(t, {}))
            if op['dsem']:
                k = ('d', op['dsem'])
                dcnt[k] = dcnt.get(k, 0) + 16
                sig = (k, dcnt[k])
            else:
                k = ('e', op['eng'])
                cnt[op['eng']] += 1
                sig = (k, cnt[op['eng']])
            op['sig'] = sig
            allsig[sig[0]] = sig[1]
            ws = []
            for k, v in need.items():
                if k == ('e', 'pe') and op['eng'] == 'pe' and not op['dsem']:
                    continue
                if waited[op['eng']].get(k, 0) < v:
                    ws.append((k, v))
                    waited[op['eng']][k] = v
            op['waits'] = ws
            for t in op['r']:
                if t in op['w']:
                    continue
                tokR.setdefault(t, {})[sig[0]] = sig[1]
            for t in op['w']:
                if tokR.get(t) or t in op['r']:
                    tokW[t] = {sig[0]: sig[1]}
                    tokR[t] = {}
                else:
                    tokW.setdefault(t, {})[sig[0]] = sig[1]
        self.final = dict(allsig)

    def emit(self):
        self.analyze()
        nc = self.nc
        keys = sorted({op['sig'][0] for op in self.ops})
        for k in keys:
            name = "s_" + "_".join(str(x) for x in k)
            self.sems[k] = self.es.enter_context(nc.semaphore(name))
        with nc.Block() as block:
            def run(name):
                def f(e):
                    for op in self.ops:
                        if op['eng'] != name:
                            continue
                        for k, v in op['waits']:
                            e.wait_ge(self.sems[k], v)
                        ins = op['fn'](e)
                        ins.then_inc(self.sems[op['sig'][0]], 16 if op['dsem'] else 1)
                    if name == 'sp':
                        for k, v in self.final.items():
                            e.wait_ge(self.sems[k], v)
                return f
            block.tensor(run('pe'))
            block.scalar(run('act'))
            block.vector(run('dve'))
            block.gpsimd(run('pool'))
            block.sync(run('sp'))


class Ctx:
    pass


def build(T, stop_after=None, with_moe=True):
    NTOK = NCTX + T
    nc = bass.Bass("TRN2", target_bir_lowering=False)
    din = lambda name, shape, dt=F32: nc.dram_tensor(name, list(shape), dt, kind="ExternalInput").ap()
    dsc = lambda name, shape, dt=F32: nc.dram_tensor(name, list(shape), dt, kind="Internal").ap()
    dout = lambda name, shape, dt=F32: nc.dram_tensor(name, list(shape), dt, kind="ExternalOutput").ap()
    I = Ctx()
    I.xT0 = din("xT0", [D, NTOK])
    I.c2 = din("c2", [128, KT, 2])
    I.ada_w = din("ada_w", [2, D, 6 * D])
    I.ada_b = din("ada_b", [2, 128, 48])
    I.gmix = din("gmix", [2, 128, KT])
    I.gffn = din("gffn", [2, 128, KT])
    I.gfin = din("gfin", [128, KT])
    I.w_in0 = din("w_in0", [D, 4096])
    I.w_out0 = din("w_out0", [D, D])
    I.lblT = din("lblT", [128, 3, 2, 4])
    I.lblrow = din("lblrow", [1, 3 * 2 * 512])
    I.onorm = din("onorm", [128, 1])
    I.subln = din("subln", [128, 1])
    I.dlam = din("dlam", [64, 4])
    I.cosT = din("cosT", [128, NTOK])
    I.sinT = din("sinT", [128, NTOK])
    I.rotm = din("rotm", [128, 128])
    I.consts = din("consts", [128, 6, 128])
    I.router_w = din("router_w", [128, KT, NE])
    I.router_b = din("router_b", [1, NE])
    if with_moe:
        I.w_gate = din("w_gate", [2, NE, D, D])
        I.w_up = din("w_up", [2, NE, D, D])
        I.w_down = din("w_down", [2, NE, D, D])
    I.w_qkv1 = din("w_qkv1", [D, 1536])
    I.w_out1 = din("w_out1", [D, D])
    I.sink = din("sink", [1, 16])
    out = dout("out", [D, T])

    es = ExitStack()
    with es:
        S = Sched(nc, es)
        ARENA = 52000
        arena = es.enter_context(nc.sbuf_tensor("arena", [128, ARENA], F32))
        apos = [0, 0]

        def sb(name, shape, dt=F32):
            free = 1
            for d_ in shape[1:]:
                free *= d_
            n32 = free if dt == F32 else (free + 1) // 2
            n32 = (n32 + 15) // 16 * 16
            off = apos[0]
            apos[0] += n32
            assert apos[0] <= ARENA, (name, apos[0])
            a = arena[0:shape[0], off:off + n32]
            if dt != F32:
                a = a.bitcast(dt)
            a = a[:, 0:free]
            if len(shape) == 3:
                a = a.rearrange("p (a b) -> p a b", b=shape[2])
            elif len(shape) == 4:
                a = a.rearrange("p (a b c) -> p a b c", b=shape[2], c=shape[3])
            elif len(shape) == 6:
                a = a.rearrange("p (a b c d e) -> p a b c d e", b=shape[2], c=shape[3], d=shape[4], e=shape[5])
            return a

        def phase_reset():
            S.barrier()
            apos[0] = apos[1]
        ps = [es.enter_context(nc.psum_tensor(f"ps{i}", [128, 512], F32)) for i in range(8)]
        P = lambda i: f"ps{i}"

        cst = sb("cst", [128, 6, 128])
        cstb = sb("cstb", [128, 6, 128], BF16)
        S.dma('sp', cst[:], I.consts[:, :, :], 'c0', (), ['cst'])
        S.copy('dve', cstb[:], cst[:], ['cst'], ['cstb'])
        IDENT, TLE, TGE, TGT, TLT, ONES = range(6)
        c2s = sb("c2s", [128, KT, 2]); sc2 = sb("sc2", [128, KT, 2])
        adab = sb("adab", [128, 2, 48]); gm = sb("gm", [128, 2, KT]); gf = sb("gf", [128, 2, KT])
        gfin = sb("gfin_s", [128, KT])
        mod = sb("mod", [128, 2, 48, 2])
        S.dma('sp', c2s[:], I.c2[:, :, :], 'c0', (), ['c2s'])
        S.dma('sp', adab[:], I.ada_b.rearrange("l p j -> p l j"), 'c0', (), ['adab'])
        S.dma('sp', gm[:], I.gmix.rearrange("l p j -> p l j"), 'c0', (), ['gm'])
        S.dma('sp', gf[:], I.gffn.rearrange("l p j -> p l j"), 'c0', (), ['gf'])
        S.dma('sp', gfin[:], I.gfin[:, :], 'c0', (), ['gfin'])
        S.act(sc2[:], c2s[:], AF.Silu, ['c2s'], ['sc2'])
        ab = sb("ab", [128, 2, 2, 2, 2, KT])
        LAMBDA_INIT = 0.8 - 0.6 * math.exp(-0.3 * 0)
        dl = sb("dl", [64, 4]); dlp = sb("dlp", [64, 2]); lam = sb("lam", [128, 2]); nlam = sb("nlam", [128, 1])
        subl = sb("subl", [128, 1]); onrm = sb("onrm", [128, 1])
        S.dma('sp', dl[:], I.dlam[:, :], 'x', (), ['dl'])
        S.dma('sp', subl[:], I.subln[:, :], 'x', (), ['subl'])
        S.dma('sp', onrm[:], I.onorm[:, :], 'x', (), ['onrm'])
        S.tt('dve', dlp[:, 0:1], dl[:, 0:1], dl[:, 1:2], ALU.mult, ['dl'], ['dlp'])
        S.tt('dve', dlp[:, 1:2], dl[:, 2:3], dl[:, 3:4], ALU.mult, ['dl'], ['dlp'])
        S.mm(ps[0][:, 0:2], cst[0:64, ONES, :], dlp[:, :], True, True, ['cst', 'dlp'], [P(0)])
        S.act(lam[:], ps[0][:, 0:2], AF.Exp, [P(0)], ['lam'])
        S.tt('dve', nlam[:], lam[:, 1:2], lam[:, 0:1], ALU.subtract, ['lam'], ['nlam'])
        S.ts('dve', nlam[:], nlam[:], -LAMBDA_INIT, None, ALU.add, None, ['nlam'], ['nlam'])
        S.ts('dve', subl[:], subl[:], 1.0 - LAMBDA_INIT, None, ALU.mult, None, ['subl'], ['subl'])

        rot = sb("rot", [128, 128]); rotb = sb("rotb", [128, 128], BF16)
        S.dma('sp', rot[:], I.rotm[:, :], 'c0', (), ['rot'])
        S.copy('dve', rotb[:], rot[:], ['rot'], ['rotb'])
        apos[1] = apos[0]
        wch = [sb(f"wch{i}", [128, KT, 768]) for i in range(2)]
        for l in range(2):
            for ch in range(8):
                buf = wch[ch % 2]; tk = f'wch{ch % 2}'
                S.dma('sp', buf[:], I.ada_w[l, :, ch * 768:(ch + 1) * 768].rearrange("(k p) n -> p k n", p=128),
                      tk, (), [tk])
                for jj in range(6):
                    j = ch * 6 + jj
                    for kt in range(KT):
                        S.mm(ps[0][:, j * 2:j * 2 + 2], buf[:, kt, jj * 128:(jj + 1) * 128], sc2[:, kt, :],
                             kt == 0, kt == KT - 1, [tk, 'sc2'], [P(0)])
            S.tt('dve', mod[:, l, :, :], ps[0][:, 0:96].rearrange("p (j t) -> p j t", t=2),
                 adab[:, l, :].unsqueeze(2).to_broadcast([128, 48, 2]), ALU.add, [P(0), 'adab'], ['mod'])
        for l in range(2):
            for wh, (gg, o) in enumerate(((gm, 0), (gf, 24))):
                for t in range(2):
                    S.stt('dve', ab[:, l, wh, t, 0, :], mod[:, l, o + 8:o + 16, t], 1.0, gg[:, l, :],
                          ALU.add, ALU.mult, ['mod', 'gm', 'gf'], ['ab'])
                    S.copy('dve', ab[:, l, wh, t, 1, :], mod[:, l, o:o + 8, t], ['mod'], ['ab'])

        NB = Ctx()

        def alloc_hn():
            NB.sqb = sb("sqb", [128, 512], BF16); NB.yb = sb("yb", [128, 512], BF16); NB.rr = sb("rr", [128, 512])

        def head_norm_store(o_ap, o_tok, gcol, gtok, dst, qb, extra_mul=None):
            S.act(NB.sqb[:, 0:qb], o_ap, AF.Square, [o_tok], ['sqb'])
            S.mm(ps[0][:, 0:qb], cstb[:, ONES, :], NB.sqb[:, 0:qb], True, True, ['cstb', 'sqb'], [P(0)])
            S.act(NB.rr[:, 0:qb], ps[0][:, 0:qb], AF.Sqrt, [P(0)], ['rr'], bias=EPS, scale=1.0 / 128)
            S.recip(NB.rr[:, 0:qb], NB.rr[:, 0:qb], ['rr'], ['rr'])
            S.stt('dve', NB.rr[:, 0:qb], o_ap, gcol, NB.rr[:, 0:qb], ALU.mult, ALU.mult, [o_tok, 'rr', gtok], ['rr'])
            if extra_mul is not None:
                S.tt('dve', NB.yb[:, 0:qb], NB.rr[:, 0:qb], extra_mul[0], ALU.mult, ['rr', extra_mul[1]], ['yb'])
            else:
                S.copy('dve', NB.yb[:, 0:qb], NB.rr[:, 0:qb], ['rr'], ['yb'])
            S.dma('sp', dst, NB.yb[:, 0:qb], 'st_yb', ['yb'], ['dram'])


        def alloc_norm():
            NB.xt = sb("xt", [128, KT, 512]); NB.sq = sb("sq", [128, KT, 512], BF16)
            NB.rstd = sb("rstd", [128, 512]); NB.hT = sb("hT", [128, KT, 512], BF16)

        def norm_block(src, t0, tb, l, wh, typ, hout=None, htok='hT', f32out=None):
            xt, sq, rstd, hT = NB.xt, NB.sq, NB.rstd, NB.hT
            S.dma('sp', xt[:, :, 0:tb], src[:, t0:t0 + tb].rearrange("(k p) n -> p k n", p=128), 'xt', (), ['xt'])
            S.act(sq[:, :, 0:tb], xt[:, :, 0:tb], AF.Square, ['xt'], ['sq'])
            for kt in range(KT):
                S.mm(ps[7][:, 0:tb], cstb[:, ONES, :], sq[:, kt, 0:tb], kt == 0, kt == KT - 1, ['cstb', 'sq'], [P(7)])
            S.act(rstd[:, 0:tb], ps[7][:, 0:tb], AF.Sqrt, [P(7)], ['rstd'], bias=EPS, scale=1.0 / D)
            S.recip(rstd[:, 0:tb], rstd[:, 0:tb], ['rstd'], ['rstd'])
            ho = hT if hout is None else hout
            for kt in range(KT):
                S.tt('dve', xt[:, kt, 0:tb], xt[:, kt, 0:tb], rstd[:, 0:tb], ALU.mult, ['xt', 'rstd'], ['xt'])
                if f32out is not None:
                    S.ts('dve', xt[:, kt, 0:tb], xt[:, kt, 0:tb], ab[:, l, wh, typ, 0, kt:kt + 1],
                         ab[:, l, wh, typ, 1, kt:kt + 1], ALU.mult, ALU.add, ['xt', 'ab'], ['xt'])
                    S.copy('pool', ho[:, kt, 0:tb], xt[:, kt, 0:tb], ['xt'], [htok])
                else:
                    S.ts('dve', ho[:, kt, 0:tb], xt[:, kt, 0:tb], ab[:, l, wh, typ, 0, kt:kt + 1],
                         ab[:, l, wh, typ, 1, kt:kt + 1], ALU.mult, ALU.add, ['xt', 'ab'], [htok])

        blocks = [(0, NCTX, 1)] + [(NCTX + i * 512, 512, 0) for i in range(T // 512)]
        phase_reset()
        alloc_norm(); hT = NB.hT

        YT = dsc("YT", [D, NTOK], BF16)
        XT1 = dsc("XT1", [D, NTOK], F32)
        hqT = dsc("hqT", [512, NTOK], BF16); hkT = dsc("hkT", [2, 512, NTOK], BF16); hgT = dsc("hgT", [512, NTOK], BF16)
        hlf = dsc("hlf", [2, NTOK, 512], F32); hk = dsc("hk", [2, NTOK, 512], BF16); hv = dsc("hv", [NTOK, 512], BF16)
        dqT = dsc("dqT", [512, NTOK], BF16); dkT = dsc("dkT", [512, NTOK], BF16); dv = dsc("dv", [NTOK, 512], BF16)

        win = sb("win", [128, KT, 4096], BF16)
        for kt in range(KT):
            S.dma('pool', win[:, kt, :], I.w_in0[kt * 128:(kt + 1) * 128, :], 'win', (), ['win'])
        lbT = sb("lbT", [128, 3, 8]); lbs = sb("lbs", [128, 8]); omlT = sb("omlT", [128, 8]); lb0T = sb("lb0T", [128, 8])
        S.dma('sp', lbT[:], I.lblT.rearrange("p l d h -> p l (d h)"), 'c0', (), ['lbT'])
        S.act(lbT[:], lbT[:], AF.Exp, ['lbT'], ['lbT'])
        S.tt('dve', lbs[:], lbT[:, 0, :], lbT[:, 1, :], ALU.add, ['lbT'], ['lbs'])
        S.tt('dve', lbs[:], lbs[:], lbT[:, 2, :], ALU.add, ['lbT', 'lbs'], ['lbs'])
        S.recip(lbs[:], lbs[:], ['lbs'], ['lbs'])
        S.tt('dve', lb0T[:], lbT[:, 0, :], lbs[:], ALU.mult, ['lbT', 'lbs'], ['lb0T'])
        S.ts('dve', omlT[:], lb0T[:], -1.0, 1.0, ALU.mult, ALU.add, ['lb0T'], ['omlT'])
        lbr = sb("lbr", [128, 3, 1024]); lbrs = sb("lbrs", [128, 1024]); lb0r = sb("lb0r", [128, 1024]); omlr = sb("omlr", [128, 1024])
        S.dma('sp', lbr[:], I.lblrow.rearrange("o (l n) -> o l n", l=3).partition_broadcast(128), 'c0', (), ['lbr'])
        S.act(lbr[:], lbr[:], AF.Exp, ['lbr'], ['lbr'])
        S.tt('dve', lbrs[:], lbr[:, 0, :], lbr[:, 1, :], ALU.add, ['lbr'], ['lbrs'])
        S.tt('dve', lbrs[:], lbrs[:], lbr[:, 2, :], ALU.add, ['lbr', 'lbrs'], ['lbrs'])
        S.recip(lbrs[:], lbrs[:], ['lbrs'], ['lbrs'])
        S.tt('dve', lb0r[:], lbr[:, 0, :], lbrs[:], ALU.mult, ['lbr', 'lbrs'], ['lb0r'])
        S.ts('dve', omlr[:], lb0r[:], -1.0, 1.0, ALU.mult, ALU.add, ['lb0r'], ['omlr'])

        cosb = sb("cosb", [128, 512]); sinb = sb("sinb", [128, 512])
        ev = sb("ev", [128, 512]); evb = sb("evb", [128, 512], BF16); ev2 = sb("ev2", [128, 512])
        otb = [sb(f"otb{i}", [128, 512], BF16) for i in range(2)]
        otf = sb("otf", [128, 512])
        nq = [0]

        def store(dst, src_ap, src_tok):
            S.dma('sp', dst, src_ap, 'st_' + src_tok, [src_tok], ['dram'])

        for (t0, tb, typ) in blocks:
            norm_block(I.xT0, t0, tb, 0, 0, typ)
            S.dma('sp', cosb[:, 0:tb], I.cosT[:, t0:t0 + tb], 'cs', (), ['cosb'])
            S.dma('sp', sinb[:, 0:tb], I.sinT[:, t0:t0 + tb], 'cs', (), ['sinb'])
            fm_jobs = []
            for h in range(4): fm_jobs.append(('q', 0 + h * 128, hqT[h * 128:(h + 1) * 128, t0:t0 + tb], h))
            for d in range(2):
                for h in range(4): fm_jobs.append(('k', 512 + d * 512 + h * 128, hkT[d, h * 128:(h + 1) * 128, t0:t0 + tb], d * 4 + h))
            for h in range(4): fm_jobs.append(('g', 2048 + h * 128, hgT[h * 128:(h + 1) * 128, t0:t0 + tb], h))
            for h in range(4): fm_jobs.append(('r', 2560 + h * 128, dqT[h * 128:(h + 1) * 128, t0:t0 + tb], h))
            for h in range(4): fm_jobs.append(('r', 3072 + h * 128, dkT[h * 128:(h + 1) * 128, t0:t0 + tb], h))
            for (kind, c0, dst, idx) in fm_jobs:
                pb = nq[0] % 2; nq[0] += 1
                pp = ps[pb]; ob = otb[pb]; otk = f'otb{pb}'
                for kt in range(KT):
                    S.mm(pp[:, 0:tb], win[:, kt, c0:c0 + 128], hT[:, kt, 0:tb], kt == 0, kt == KT - 1, ['win', 'hT'], [P(pb)])
                if kind in ('q', 'g'):
                    S.act(ob[:, 0:tb], pp[:, 0:tb], AF.Silu, [P(pb)], [otk])
                elif kind == 'k':
                    S.act(ev[:, 0:tb], pp[:, 0:tb], AF.Sigmoid, [P(pb)], ['ev'], scale=-1.0)
                    S.ts('dve', ob[:, 0:tb], ev[:, 0:tb], omlT[:, idx:idx + 1], None, ALU.mult, None, ['ev', 'omlT'], [otk])
                else:
                    S.copy('act', evb[:, 0:tb], pp[:, 0:tb], [P(pb)], ['evb'])
                    S.mm(ps[2][:, 0:tb], rotb[:], evb[:, 0:tb], True, True, ['rotb', 'evb'], [P(2)])
                    S.tt('dve', ev[:, 0:tb], evb[:, 0:tb], cosb[:, 0:tb], ALU.mult, ['evb', 'cosb'], ['ev'])
                    S.tt('dve', ev2[:, 0:tb], ps[2][:, 0:tb], sinb[:, 0:tb], ALU.mult, [P(2), 'sinb'], ['ev2'])
                    S.tt('dve', ob[:, 0:tb], ev[:, 0:tb], ev2[:, 0:tb], ALU.add, ['ev', 'ev2'], [otk])
                store(dst, ob[:, 0:tb], otk)
            for tt_ in range(tb // 128):
                n0 = t0 + tt_ * 128
                for gi, (kind, c0) in enumerate((('f', 512), ('f', 1024), ('c', 1536), ('c', 3584))):
                    pb = 3 + gi % 2; pp = ps[pb]
                    for kt in range(KT):
                        S.mm(pp[:, :], hT[:, kt, tt_ * 128:(tt_ + 1) * 128], win[:, kt, c0:c0 + 512],
                             kt == 0, kt == KT - 1, ['win', 'hT'], [P(pb)])
                    if kind == 'f':
                        d = gi
                        S.act(ev[:, :], pp[:, :], AF.Sigmoid, [P(pb)], ['ev'])
                        S.tt('dve', ev[:, :], ev[:, :], omlr[:, d * 512:(d + 1) * 512], ALU.mult, ['ev', 'omlr'], ['ev'])
                        S.tt('dve', ev[:, :], ev[:, :], lb0r[:, d * 512:(d + 1) * 512], ALU.add, ['ev', 'lb0r'], ['ev'])
                        S.act(otf[:, :], ev[:, :], AF.Ln, ['ev'], ['otf'])
                        store(hlf[d, n0:n0 + 128, :], otf[:, :], 'otf')
                        S.ts('dve', otb[0][:, :], ev[:, :], -1.0, 1.0, ALU.mult, ALU.add, ['ev'], ['otb0'])
                        store(hk[d, n0:n0 + 128, :], otb[0][:, :], 'otb0')
                    else:
                        S.copy('act', otb[1][:, :], pp[:, :], [P(pb)], ['otb1'])
                        store((hv if c0 == 1536 else dv)[n0:n0 + 128, :], otb[1][:, :], 'otb1')
        phase_reset()
        if stop_after == 'A':
            dbg = dout("dbg_hlf", [2, NTOK, 512]); dbg2 = dout("dbg_dkT", [512, NTOK], BF16)
            dbg3 = dout("dbg_hkT", [2, 512, NTOK], BF16)
            S.dma('sp', dbg[:, :, :], hlf[:, :, :], 'dbg', ['dram'], ['dbg'])
            S.dma('sp', dbg2[:, :], dkT[:, :], 'dbg', ['dram'], ['dbg'])
            S.dma('sp', dbg3[:, :, :], hkT[:, :, :], 'dbg', ['dram'], ['dbg'])
            S.emit()
            return nc

        NCH = NTOK // HC
        NCC = NCTX // HC
        LNS = math.log(128 ** -0.5)
        CAP = max(NCC, (NCH + 1) // 2)
        qTh = sb("qTh", [128, NTOK], BF16); vth = sb("vth", [64, CAP, 128], BF16)
        kTh = sb("kTh", [128, CAP * HC], BF16); kth = sb("kth", [64, CAP, 128], BF16)
        lfh = sb("lfh", [64, CAP, 128]); Oacc = sb("Oacc", [128, NTOK]); gTh = sb("gTh", [128, 512], BF16)
        Sst = sb("Sst", [128, 128])
        alloc_hn()
        HB = []
        for i in range(2):
            c_ = Ctx()
            c_.eq = sb(f"eq{i}", [128, 64]); c_.ek = sb(f"ek{i}", [128, 64]); c_.er = sb(f"er{i}", [64, 128])
            c_.QsT = sb(f"QsT{i}", [128, 64], BF16); c_.KsT = sb(f"KsT{i}", [128, 64], BF16)
            c_.Kout = sb(f"Kout{i}", [64, 128], BF16); c_.ATm = sb(f"ATm{i}", [64, 64], BF16)
            c_.Smid = sb(f"Smid{i}", [128, 128], BF16); c_.sc = sb(f"hsc{i}", [128, 4])
            HB.append(c_)
        for h in range(4):
            hs_ = slice(h * 128, (h + 1) * 128)
            S.dma('sp', qTh[:], hqT[hs_, :], 'x', ['dram'], ['qTh'])
            for d in range(2):
                MC, MR = (TLE, TGT) if d == 0 else (TGE, TLT)
                S.memset('dve', Sst[:], 0.0, ['Sst'])
                lohi = [None]
                if d == 0:
                    order = list(range(NCH))
                else:
                    order = list(range(NCC - 1, -1, -1)) + list(range(NCH - 1, NCC - 1, -1))
                for ci, c_abs in enumerate(order):
                    if lohi[0] is None or not (lohi[0][0] <= c_abs < lohi[0][1]):
                        run = [c_abs]
                        for c2 in order[ci + 1:]:
                            lo_, hi_ = min(run + [c2]), max(run + [c2]) + 1
                            if hi_ - lo_ > CAP:
                                break
                            run.append(c2)
                        lo_, hi_ = min(run), max(run) + 1
                        lohi[0] = (lo_, hi_)
                        nch_ = hi_ - lo_
                        S.dma('sp', kTh[:, 0:nch_ * HC], hkT[d, hs_, lo_ * HC:hi_ * HC], 'x', ['dram'], ['kTh'])
                        S.dma('sp', kth[:, 0:nch_, :], hk[d, lo_ * HC:hi_ * HC, :].rearrange("(n p) c -> p n c", p=HC)[:, :, hs_],
                              'x', ['dram'], ['kth'])
                        S.dma('sp', lfh[:, 0:nch_, :], hlf[d, lo_ * HC:hi_ * HC, :].rearrange("(n p) c -> p n c", p=HC)[:, :, hs_],
                              'x', ['dram'], ['lfh'])
                        S.dma('sp', vth[:, 0:nch_, :], hv[lo_ * HC:hi_ * HC, :].rearrange("(n p) c -> p n c", p=HC)[:, :, hs_],
                              'x', ['dram'], ['vth'])
                    c = c_abs - lohi[0][0]
                    B_ = HB[ci % 2]; st = ci % 2
                    pA, pB, pD, pE = ps[4 * st], ps[4 * st + 1], ps[4 * st + 2], ps[4 * st + 3]
                    tA, tB, tD, tE = P(4 * st), P(4 * st + 1), P(4 * st + 2), P(4 * st + 3)
                    sfx = str(st)
                    cs = slice(c_abs * HC, (c_abs + 1) * HC)
                    csl = slice(c * HC, (c + 1) * HC)
                    S.mm(pA[:, 0:64], lfh[:, c, :], cst[0:64, MC, 0:64], True, True, ['lfh', 'cst'], [tA])
                    S.mm(pB[0:64, 0:128], cst[0:64, MR, 0:64], lfh[:, c, :], True, True, ['lfh', 'cst'], [tB])
                    tcol = 63 if d == 0 else 0
                    S.ts('dve', B_.sc[:, 0:1], pA[:, 31:32], -1.0, LNS, ALU.mult, ALU.add, [tA], ['hsc' + sfx])
                    S.copy('dve', B_.sc[:, 1:2], pA[:, 31:32], [tA], ['hsc' + sfx])
                    S.act(B_.sc[:, 2:3], pA[:, tcol:tcol + 1], AF.Exp, [tA], ['hsc' + sfx])
                    S.act(B_.sc[:, 3:4], pA[:, 31:32], AF.Exp, [tA], ['hsc' + sfx])
                    S.act(B_.eq[:], pA[:, 0:64], AF.Exp, [tA, 'hsc' + sfx], ['eq' + sfx], bias=B_.sc[:, 0:1], scale=1.0)
                    S.act(B_.ek[:], pA[:, 0:64], AF.Exp, [tA, 'hsc' + sfx], ['ek' + sfx], bias=B_.sc[:, 1:2], scale=-1.0)
                    S.act(B_.er[:], pB[0:64, 0:128], AF.Exp, [tB], ['er' + sfx])
                    S.tt('dve', B_.QsT[:], qTh[:, cs], B_.eq[:], ALU.mult, ['qTh', 'eq' + sfx], ['QsT' + sfx])
                    S.tt('dve', B_.KsT[:], kTh[:, csl], B_.ek[:], ALU.mult, ['kTh', 'ek' + sfx], ['KsT' + sfx])
                    S.tt('dve', B_.Kout[:], kth[:, c, :], B_.er[:], ALU.mult, ['kth', 'er' + sfx], ['Kout' + sfx])
                    S.mm(pB[0:64, 128:192], B_.KsT[:], B_.QsT[:], True, True, ['KsT' + sfx, 'QsT' + sfx], [tB])
                    S.tt('dve', B_.ATm[:], pB[0:64, 128:192], cst[0:64, MC, 0:64], ALU.mult, [tB, 'cst'], ['ATm' + sfx])
                    S.ts('dve', B_.Smid[:], Sst[:], B_.sc[:, 3:4], None, ALU.mult, None, ['Sst', 'hsc' + sfx], ['Smid' + sfx])
                    S.mm(pD[:, 0:64], vth[:, c, :], B_.ATm[:], True, False, ['vth', 'ATm' + sfx], [tD])
                    S.mm(pD[:, 0:64], B_.Smid[:], B_.QsT[:], False, True, ['Smid' + sfx, 'QsT' + sfx], [tD])
                    if d == 0:
                        S.copy('act', Oacc[:, cs], pD[:, 0:64], [tD], ['Oacc'])
                    else:
                        S.tt('dve', Oacc[:, cs], pD[:, 0:64], Oacc[:, cs], ALU.add, [tD, 'Oacc'], ['Oacc'])
                    S.mm(pE[:, 0:128], B_.Kout[:], vth[:, c, :], True, True, ['Kout' + sfx, 'vth'], [tE])
                    S.stt('dve', Sst[:], Sst[:], B_.sc[:, 2:3], pE[:, 0:128], ALU.mult, ALU.add,
                          ['Sst', 'hsc' + sfx, tE], ['Sst'])
            for (t0, tb, typ) in blocks:
                S.dma('sp', gTh[:, 0:tb], hgT[hs_, t0:t0 + tb], 'x', ['dram'], ['gTh'])
                head_norm_store(Oacc[:, t0:t0 + tb], 'Oacc', onrm[:, 0:1], 'onrm', YT[hs_, t0:t0 + tb], tb,
                                extra_mul=(gTh[:, 0:tb], 'gTh'))
        phase_reset()
        if stop_after == 'B':
            dbg = dout("dbg_YT", [D, NTOK], BF16)
            S.dma('sp', dbg[:, :], YT[:, :], 'x', ['dram'], ['dbg'])
            S.emit()
            return nc

        NKT = NTOK // 128
        KTh = sb("KTh", [128, NTOK], BF16); Vh = sb("Vh", [128, NKT, 128], BF16)
        QTh = sb("QTh", [128, 512], BF16)
        e1 = [sb(f"e1_{i}", [128, 512], BF16) for i in range(2)]
        e2 = [sb(f"e2_{i}", [128, 512], BF16) for i in range(2)]
        oa = sb("oa", [128, 512]); ob2 = sb("ob2", [128, 512])
        alloc_hn(); rr = NB.rr

        for h in range(4):
            S.dma('sp', KTh[:], dkT[h * 128:(h + 1) * 128, :], 'x', ['dram'], ['KTh'])
            S.dma('sp', Vh[:], dv.rearrange("(n p) c -> p n c", p=128)[:, :, h * 128:(h + 1) * 128], 'x', ['dram'], ['Vh'])
            qblocks = [(0, NCTX, 0, NCTX // 128)] + [(NCTX + i * 512, 512, 0, NKT) for i in range(T // 512)]
            for (q0, qb, k0, k1) in qblocks:
                S.dma('sp', QTh[:, 0:qb], dqT[h * 128:(h + 1) * 128, q0:q0 + qb], 'x', ['dram'], ['QTh'])
                for ki, kt_ in enumerate(range(k0, k1)):
                    a = ki % 2
                    sA, sB = (0, 1) if a == 0 else (6, 7)
                    ks = slice(kt_ * 128, (kt_ + 1) * 128)
                    S.mm(ps[sA][:, 0:qb], KTh[0:64, ks], QTh[0:64, 0:qb], True, True, ['KTh', 'QTh'], [P(sA)])
                    S.mm(ps[sB][:, 0:qb], KTh[64:128, ks], QTh[64:128, 0:qb], True, True, ['KTh', 'QTh'], [P(sB)])
                    S.act(e1[a][:, 0:qb], ps[sA][:, 0:qb], AF.Exp, [P(sA)], [f'e1_{a}'], scale=0.125)
                    S.act(e2[a][:, 0:qb], ps[sB][:, 0:qb], AF.Exp, [P(sB)], [f'e2_{a}'], scale=0.125)
                    first, last = (kt_ == k0), (kt_ == k1 - 1)
                    S.mm(ps[2][:, 0:qb], Vh[:, kt_, :], e1[a][:, 0:qb], first, last, ['Vh', f'e1_{a}'], [P(2)])
                    S.mm(ps[3][:, 0:qb], Vh[:, kt_, :], e2[a][:, 0:qb], first, last, ['Vh', f'e2_{a}'], [P(3)])
                    S.mm(ps[4][:, 0:qb], cstb[:, ONES, :], e1[a][:, 0:qb], first, last, ['cstb', f'e1_{a}'], [P(4)])
                    S.mm(ps[5][:, 0:qb], cstb[:, ONES, :], e2[a][:, 0:qb], first, last, ['cstb', f'e2_{a}'], [P(5)])
                S.recip(rr[:, 0:qb], ps[4][:, 0:qb], [P(4)], ['rr'])
                S.tt('dve', oa[:, 0:qb], ps[2][:, 0:qb], rr[:, 0:qb], ALU.mult, [P(2), 'rr'], ['oa'])
                S.recip(rr[:, 0:qb], ps[5][:, 0:qb], [P(5)], ['rr'])
                S.tt('dve', ob2[:, 0:qb], ps[3][:, 0:qb], rr[:, 0:qb], ALU.mult, [P(3), 'rr'], ['ob2'])
                S.stt('dve', oa[:, 0:qb], ob2[:, 0:qb], nlam[:, 0:1], oa[:, 0:qb], ALU.mult, ALU.add, ['ob2', 'nlam', 'oa'], ['oa'])
                head_norm_store(oa[:, 0:qb], 'oa', subl[:, 0:1], 'subl', YT[512 + h * 128:512 + (h + 1) * 128, q0:q0 + qb], qb)
        phase_reset()
        if stop_after == 'C':
            dbg = dout("dbg_YT", [D, NTOK], BF16)
            S.dma('sp', dbg[:, :], YT[:, :], 'x', ['dram'], ['dbg'])
            S.emit()
            return nc

        XT2 = dsc("XT2", [D, NTOK], F32)
        XT3 = dsc("XT3", [D, NTOK], F32)
        XT4 = dsc("XT4", [D, NTOK], F32)
        WRT = dsc("WRT", [NE, NTOK], F32)

        def outproj_phase(l, w_dram, xsrc, xdst, blks):
            wo = sb("wo", [128, KT, D], BF16)
            for kt in range(KT):
                S.dma('pool', wo[:, kt, :], w_dram[kt * 128:(kt + 1) * 128, :], 'x', (), ['wo'])
            Yb = sb("Yb", [128, KT, 512], BF16); xo = sb("xo", [128, KT, 512])
            for (t0, tb, typ) in blks:
                S.dma('sp', Yb[:, :, 0:tb], YT[:, t0:t0 + tb].rearrange("(k p) n -> p k n", p=128), 'x', ['dram'], ['Yb'])
                S.dma('sp', xo[:, :, 0:tb], xsrc[:, t0:t0 + tb].rearrange("(k p) n -> p k n", p=128), 'x', ['dram'], ['xo'])
                for dt_ in range(KT):
                    pb = dt_ % 2
                    for kt in range(KT):
                        S.mm(ps[pb][:, 0:tb], wo[:, kt, dt_ * 128:(dt_ + 1) * 128], Yb[:, kt, 0:tb],
                             kt == 0, kt == KT - 1, ['wo', 'Yb'], [P(pb)])
                    S.stt('dve', xo[:, dt_, 0:tb], ps[pb][:, 0:tb], mod[:, l, 16 + dt_, typ:typ + 1], xo[:, dt_, 0:tb],
                          ALU.mult, ALU.add, [P(pb), 'mod', 'xo'], ['xo'])
                S.dma('sp', xdst[:, t0:t0 + tb].rearrange("(k p) n -> p k n", p=128), xo[:, :, 0:tb], 'st_xo', ['xo'], ['dram'])
            phase_reset()

        outproj_phase(0, I.w_out0, I.xT0, XT1, blocks)
        if stop_after == 'D':
            dbg = dout("dbg_X", [D, NTOK], F32)
            S.dma('sp', dbg[:, :], XT1[:, :], 'x', ['dram'], ['dbg'])
            S.emit()
            return nc

        def moe_phase(l, xsrc, xdst, blks):
            NB.xt = sb("xt", [128, KT, 512]); NB.sq = sb("sq", [128, KT, 512], BF16)
            NB.rstd = sb("rstd", [128, 512]); NB.hT = None
            SBMAX = 1024
            rw = sb("rw", [128, KT, NE]); rb = sb("rb", [128, NE])
            S.dma('sp', rw[:], I.router_w[:, :, :], 'x', (), ['rw'])
            S.dma('sp', rb[:], I.router_b.partition_broadcast(128), 'x', (), ['rb'])
            h2 = sb("h2", [128, KT, SBMAX], BF16); xacc = sb("xacc", [128, KT, SBMAX])
            Re = sb("Re", [128, SBMAX]); WT = sb("WT", [NE, 512])
            wsc = {}
            stg = [sb(f"wstg{i}", [128, D], BF16) for i in range(2)]
            si = [0]
            for nm, src in (('g', I.w_gate), ('u', I.w_up), ('d', I.w_down)):
                wsc[nm] = dsc(f"wbf_{nm}{l}", [NE, D, D], BF16)
                for e_ in range(NE):
                    for kt in range(KT):
                        b_ = si[0] % 2; si[0] += 1
                        S.dma('pool', stg[b_][:], src[l, e_, kt * 128:(kt + 1) * 128, :], 'x', (), [f'wstg{b_}'])
                        S.dma('sp', wsc[nm][e_, kt * 128:(kt + 1) * 128, :], stg[b_][:], f'st_wstg{b_}', [f'wstg{b_}'],
                              [f'wsc_{nm}{l}'])
            wbuf = {}
            for nm in ('g', 'u', 'd'):
                for i in range(2):
                    wbuf[nm, i] = sb(f"w{nm}{i}", [128, KT, D], BF16)
            hid = sb("hid", [128, KT, 512], BF16); sg = sb("sg", [128, 512]); tmp = sb("tmpm", [128, 512])
            r_aff = sb("r_aff", [128, NE]); r_sel = sb("r_sel", [128, NE]); r_eq = sb("r_eq", [128, NE])
            r_s2 = sb("r_s2", [128, NE]); r_m = sb("r_m", [128, 12]); r_w = sb("r_w", [128, NE])
            g3 = lambda a: a[:, :].rearrange("p (g e) -> p g e", e=4)
            sbs = []; cur = []; n = 0
            for b_ in blks:
                if n + b_[1] > SBMAX:
                    sbs.append(cur); cur = []; n = 0
                cur.append(b_); n += b_[1]
            sbs.append(cur)
            ecount = [0]
            for sblk in sbs:
                n0 = sblk[0][0]; ntot = sum(b_[1] for b_ in sblk)
                off = 0
                for (t0, tb, typ) in sblk:
                    norm_block(xsrc, t0, tb, l, 1, typ, hout=h2[:, :, off:off + tb], htok='h2', f32out=True)
                    S.dma('sp', xacc[:, :, off:off + tb], xsrc[:, t0:t0 + tb].rearrange("(k p) n -> p k n", p=128),
                          'x', ['dram'], ['xacc'])
                    xt = NB.xt
                    for tt_ in range(tb // 128):
                        ts_ = slice(tt_ * 128, (tt_ + 1) * 128)
                        for kt in range(KT):
                            S.mm(ps[6][:, 0:NE], xt[:, kt, ts_], rw[:, kt, :], kt == 0, kt == KT - 1, ['xt', 'rw'], [P(6)])
                        S.act(r_aff[:], ps[6][:, 0:NE], AF.Sigmoid, [P(6)], ['r_aff'])
                        S.tt('dve', r_sel[:], r_aff[:], rb[:], ALU.add, ['r_aff', 'rb'], ['r_sel'])
                        S.reduce('dve', r_m[:, 0:4], g3(r_sel), ALU.max, ['r_sel'], ['r_m'])
                        S.tt('dve', g3(r_eq), g3(r_sel), r_m[:, 0:4].unsqueeze(2).to_broadcast([128, 4, 4]), ALU.is_equal,
                             ['r_sel', 'r_m'], ['r_eq'])
                        S.stt('dve', r_s2[:], r_eq[:], -1.0e9, r_sel[:], ALU.mult, ALU.add, ['r_eq', 'r_sel'], ['r_s2'])
                        S.reduce('dve', r_m[:, 4:8], g3(r_s2), ALU.max, ['r_s2'], ['r_m'])
                        S.tt('dve', r_m[:, 8:12], r_m[:, 0:4], r_m[:, 4:8], ALU.add, ['r_m'], ['r_m'])
                        S.reduce('dve', r_m[:, 0:1], r_m[:, 8:12], ALU.max, ['r_m'], ['r_m'])
                        S.ts('dve', r_m[:, 8:12], r_m[:, 8:12], r_m[:, 0:1], None, ALU.is_equal, None, ['r_m'], ['r_m'])
                        S.tt('dve', g3(r_eq), g3(r_sel), r_m[:, 4:8].unsqueeze(2).to_broadcast([128, 4, 4]), ALU.is_ge,
                             ['r_sel', 'r_m'], ['r_eq'])
                        S.tt('dve', g3(r_eq), g3(r_eq), r_m[:, 8:12].unsqueeze(2).to_broadcast([128, 4, 4]), ALU.mult,
                             ['r_eq', 'r_m'], ['r_eq'])
                        S.tt('dve', r_w[:], r_aff[:], r_eq[:], ALU.mult, ['r_aff', 'r_eq'], ['r_w'])
                        S.reduce('dve', r_m[:, 1:2], r_w[:], ALU.add, ['r_w'], ['r_m'])
                        S.recip(r_m[:, 1:2], r_m[:, 1:2], ['r_m'], ['r_m'])
                        S.ts('dve', r_w[:], r_w[:], r_m[:, 1:2], None, ALU.mult, None, ['r_w', 'r_m'], ['r_w'])
                        S.mm(ps[7][0:NE, 0:128], r_w[:], cst[:, IDENT, :], True, True, ['r_w', 'cst'], [P(7)])
                        S.copy('act', WT[:, ts_], ps[7][0:NE, 0:128], [P(7)], ['WT'])
                    S.dma('sp', WRT[:, t0:t0 + tb], WT[:, 0:tb], 'st_WT', ['WT'], ['dramW'])
                    off += tb
                for e in range(NE):
                    bi = ecount[0] % 2; ecount[0] += 1
                    wg, wu, wd = wbuf['g', bi], wbuf['u', bi], wbuf['d', bi]
                    tg, tu, td = f'wg{bi}', f'wu{bi}', f'wd{bi}'
                    for kt in range(KT):
                        S.dma('sp', wg[:, kt, :], wsc['g'][e, kt * 128:(kt + 1) * 128, :], 'x', [f'wsc_g{l}'], [tg])
                        S.dma('sp', wu[:, kt, :], wsc['u'][e, kt * 128:(kt + 1) * 128, :], 'x', [f'wsc_u{l}'], [tu])
                        S.dma('sp', wd[:, kt, :], wsc['d'][e, kt * 128:(kt + 1) * 128, :], 'x', [f'wsc_d{l}'], [td])
                    S.dma('sp', Re[:, 0:ntot], WRT[e:e + 1, n0:n0 + ntot].partition_broadcast(128), 'x', ['dramW'], ['Re'])
                    off = 0
                    for (t0, tb, typ) in sblk:
                        for nt in range(KT):
                            a = nt % 2
                            for kt in range(KT):
                                S.mm(ps[a][:, 0:tb], wg[:, kt, nt * 128:(nt + 1) * 128], h2[:, kt, off:off + tb],
                                     kt == 0, kt == KT - 1, [tg, 'h2'], [P(a)])
                            for kt in range(KT):
                                S.mm(ps[2 + a][:, 0:tb], wu[:, kt, nt * 128:(nt + 1) * 128], h2[:, kt, off:off + tb],
                                     kt == 0, kt == KT - 1, [tu, 'h2'], [P(2 + a)])
                            S.act(sg[:, 0:tb], ps[a][:, 0:tb], AF.Silu, [P(a)], ['sg'])
                            S.tt('dve', hid[:, nt, 0:tb], ps[2 + a][:, 0:tb], sg[:, 0:tb], ALU.mult, [P(2 + a), 'sg'], ['hid'])
                        for dt_ in range(KT):
                            a = 4 + dt_ % 2
                            for nt in range(KT):
                                S.mm(ps[a][:, 0:tb], wd[:, nt, dt_ * 128:(dt_ + 1) * 128], hid[:, nt, 0:tb],
                                     nt == 0, nt == KT - 1, [td, 'hid'], [P(a)])
                            S.stt('dve', tmp[:, 0:tb], ps[a][:, 0:tb], mod[:, l, 40 + dt_, typ:typ + 1], Re[:, off:off + tb],
                                  ALU.mult, ALU.mult, [P(a), 'mod', 'Re'], ['tmpm'])
                            S.tt('pool', xacc[:, dt_, off:off + tb], xacc[:, dt_, off:off + tb], tmp[:, 0:tb], ALU.add,
                                 ['xacc', 'tmpm'], ['xacc'])
                        off += tb
                S.dma('sp', xdst[:, n0:n0 + ntot].rearrange("(k p) n -> p k n", p=128), xacc[:, :, 0:ntot],
                      'st_xacc', ['xacc'], ['dram'])
            phase_reset()

        if with_moe:
            moe_phase(0, XT1, XT2, blocks)
        else:
            XT2 = XT1
        if stop_after == 'E':
            dbg = dout("dbg_X", [D, NTOK], F32)
            S.dma('sp', dbg[:, :], XT2[:, :], 'x', ['dram'], ['dbg'])
            S.emit()
            return nc

        latblocks = blocks[1:]
        qT1 = dsc("qT1", [D, NTOK], BF16); kT1 = dsc("kT1", [256, NTOK], BF16); v1 = dsc("v1", [NTOK, 256], BF16)
        alloc_norm(); hT = NB.hT
        wq = sb("wq", [128, KT, 1536], BF16)
        for kt in range(KT):
            S.dma('pool', wq[:, kt, :], I.w_qkv1[kt * 128:(kt + 1) * 128, :], 'x', (), ['wq'])
        cosb = sb("cosb1", [128, 512]); sinb = sb("sinb1", [128, 512])
        ev = sb("ev1", [128, 512]); evb = sb("evb1", [128, 512], BF16); ev2 = sb("ev21", [128, 512])
        otb = [sb(f"otb1_{i}", [128, 512], BF16) for i in range(2)]
        for (t0, tb, typ) in blocks:
            norm_block(XT2, t0, tb, 1, 0, typ)
            S.dma('sp', cosb[:, 0:tb], I.cosT[:, t0:t0 + tb], 'x', (), ['cosb1'])
            S.dma('sp', sinb[:, 0:tb], I.sinT[:, t0:t0 + tb], 'x', (), ['sinb1'])
            for ct in range(10):
                pb = ct % 2; pp = ps[pb]; ob = otb[pb]; otk = f'otb1_{pb}'
                for kt in range(KT):
                    S.mm(pp[:, 0:tb], wq[:, kt, ct * 128:(ct + 1) * 128], hT[:, kt, 0:tb], kt == 0, kt == KT - 1, ['wq', 'hT'], [P(pb)])
                S.copy('act', evb[:, 0:tb], pp[:, 0:tb], [P(pb)], ['evb1'])
                S.mm(ps[2][:, 0:tb], rotb[:], evb[:, 0:tb], True, True, ['rotb', 'evb1'], [P(2)])
                S.tt('dve', ev[:, 0:tb], evb[:, 0:tb], cosb[:, 0:tb], ALU.mult, ['evb1', 'cosb1'], ['ev1'])
                S.tt('dve', ev2[:, 0:tb], ps[2][:, 0:tb], sinb[:, 0:tb], ALU.mult, [P(2), 'sinb1'], ['ev21'])
                S.tt('dve', ob[:, 0:tb], ev[:, 0:tb], ev2[:, 0:tb], ALU.add, ['ev1', 'ev21'], [otk])
                dst = qT1[ct * 128:(ct + 1) * 128, t0:t0 + tb] if ct < 8 else kT1[(ct - 8) * 128:(ct - 7) * 128, t0:t0 + tb]
                S.dma('sp', dst, ob[:, 0:tb], 'st_' + otk, [otk], ['dram'])
            for tt_ in range(tb // 128):
                n0 = t0 + tt_ * 128
                for kt in range(KT):
                    S.mm(ps[3][:, 0:256], hT[:, kt, tt_ * 128:(tt_ + 1) * 128], wq[:, kt, 1280:1536], kt == 0, kt == KT - 1,
                         ['wq', 'hT'], [P(3)])
                S.copy('act', otb[0][:, 0:256], ps[3][:, 0:256], [P(3)], ['otb1_0'])
                S.dma('sp', v1[n0:n0 + 128, :], otb[0][:, 0:256], 'st_otb1_0', ['otb1_0'], ['dram'])
        phase_reset()

        NKT = NTOK // 128
        NQB = T // 128
        kT4 = sb("kT4", [64, NTOK], BF16); q4 = sb("q4", [64, 4, NTOK], BF16); v4 = sb("v4", [128, NKT, 64], BF16)
        esk = sb("esk", [64, 16])
        S.dma('sp', esk[:], I.sink.partition_broadcast(64), 'x', (), ['esk'])
        S.act(esk[:], esk[:], AF.Exp, ['esk'], ['esk'])
        eb = [sb(f"eb{i}", [128, 4, 128], BF16) for i in range(2)]
        ef = [sb(f"ef{i}", [128, 4, 128]) for i in range(2)]
        den = sb("den", [64, 4, 128]); o4 = sb("o4", [64, 4, 128], BF16)
        for kvh in range(4):
            S.dma('sp', kT4[:], kT1[kvh * 64:(kvh + 1) * 64, :], 'x', ['dram'], ['kT4'])
            S.dma('sp', q4[:], qT1[kvh * 256:(kvh + 1) * 256, :].rearrange("(g d) n -> d g n", d=64), 'x', ['dram'], ['q4'])
            S.dma('sp', v4[:], v1.rearrange("(n p) c -> p n c", p=128)[:, :, kvh * 64:(kvh + 1) * 64], 'x', ['dram'], ['v4'])
            for n in range(NQB):
                qc = slice(NCTX + n * 128, NCTX + (n + 1) * 128)
                tiles = [(0, None), (1, None)]
                if n > 0: tiles.append((2 + n - 1, TGE))
                tiles.append((2 + n, None))
                if n < NQB - 1: tiles.append((2 + n + 1, TLE))
                pso, psd = ps[4 + (n % 2) * 2], ps[5 + (n % 2) * 2]
                to, td = P(4 + (n % 2) * 2), P(5 + (n % 2) * 2)
                for ki, (kt_, msk) in enumerate(tiles):
                    a = ki % 2
                    S.mm(ps[a][:, :], kT4[:, kt_ * 128:(kt_ + 1) * 128], q4[:, :, qc], True, True, ['kT4', 'q4'], [P(a)])
                    first, last = ki == 0, ki == len(tiles) - 1
                    if msk is None:
                        S.act(eb[a][:], ps[a][:, :].rearrange("p (g n) -> p g n", g=4), AF.Exp, [P(a)], [f'eb{a}'], scale=0.125)
                    else:
                        S.act(ef[a][:], ps[a][:, :].rearrange("p (g n) -> p g n", g=4), AF.Exp, [P(a)], [f'ef{a}'], scale=0.125)
                        S.tt('dve', eb[a][:], ef[a][:], cst[:, msk, :].unsqueeze(1).to_broadcast([128, 4, 128]), ALU.mult,
                             [f'ef{a}', 'cst'], [f'eb{a}'])
                    S.mm(pso[0:64, :], v4[:, kt_, :], eb[a][:], first, last, ['v4', f'eb{a}'], [to])
                    S.mm(psd[0:64, :], cstb[:, ONES, 0:64], eb[a][:], first, last, ['cstb', f'eb{a}'], [td])
                for g in range(4):
                    hq = kvh * 4 + g
                    S.ts('dve', den[:, g, :], psd[0:64, g * 128:(g + 1) * 128], esk[:, hq:hq + 1], None, ALU.add, None,
                         [td, 'esk'], ['den'])
                S.recip(den[:], den[:], ['den'], ['den'])
                S.tt('dve', o4[:], pso[0:64, :].rearrange("p (g n) -> p g n", g=4), den[:], ALU.mult, [to, 'den'], ['o4'])
                S.dma('sp', YT[kvh * 256:(kvh + 1) * 256, qc].rearrange("(g d) n -> d g n", d=64), o4[:], 'st_o4', ['o4'], ['dram'])
        phase_reset()
        if stop_after == 'G':
            dbg = dout("dbg_YT", [D, NTOK], BF16)
            S.dma('sp', dbg[:, :], YT[:, :], 'x', ['dram'], ['dbg'])
            S.emit()
            return nc

        outproj_phase(1, I.w_out1, XT2, XT3, latblocks)
        if with_moe:
            moe_phase(1, XT3, XT4, latblocks)
        else:
            XT4 = XT3

        alloc_norm()
        xt, sq, rstd = NB.xt, NB.sq, NB.rstd
        for (t0, tb, typ) in latblocks:
            S.dma('sp', xt[:, :, 0:tb], XT4[:, t0:t0 + tb].rearrange("(k p) n -> p k n", p=128), 'x', ['dram'], ['xt'])
            S.act(sq[:, :, 0:tb], xt[:, :, 0:tb], AF.Square, ['xt'], ['sq'])
            for kt in range(KT):
                S.mm(ps[7][:, 0:tb], cstb[:, ONES, :], sq[:, kt, 0:tb], kt == 0, kt == KT - 1, ['cstb', 'sq'], [P(7)])
            S.act(rstd[:, 0:tb], ps[7][:, 0:tb], AF.Sqrt, [P(7)], ['rstd'], bias=EPS, scale=1.0 / D)
            S.recip(rstd[:, 0:tb], rstd[:, 0:tb], ['rstd'], ['rstd'])
            for kt in range(KT):
                S.stt('dve', xt[:, kt, 0:tb], xt[:, kt, 0:tb], gfin[:, kt:kt + 1], rstd[:, 0:tb], ALU.mult, ALU.mult,
                      ['xt', 'rstd', 'gfin'], ['xt'])
            S.dma('sp', out[:, t0 - NCTX:t0 - NCTX + tb].rearrange("(k p) n -> p k n", p=128), xt[:, :, 0:tb], 'st_xt', ['xt'], ['out'])
        S.emit()
    return nc


def rope_tables(T):
    gw = 64
    n_rows = T // gw
    row = np.repeat(np.arange(n_rows), gw).astype(np.float32)
    col = np.tile(np.arange(gw), n_rows).astype(np.float32)
    half = 32
    inv = (1.0 / (np.float32(10000.0) ** (np.arange(0, half, 2, dtype=np.float32) / np.float32(half)))).astype(np.float32)
    ar = row[:, None] * inv
    ac = col[:, None] * inv
    ang = np.concatenate([ar, ar, ac, ac], axis=-1)
    return np.cos(ang).astype(np.float32), np.sin(ang).astype(np.float32)


def fm(v):
    return np.ascontiguousarray(v.reshape(KT, 128).T)


def prep_inputs(inp, b, T, with_moe=True):
    NTOK = NCTX + T
    m = {}
    m["xT0"] = np.ascontiguousarray(np.concatenate([inp["ctx"][b], inp["x"][b]], axis=0).T)
    cc = np.stack([inp["c"][b], inp["c_ctx"]], axis=-1)
    m["c2"] = np.ascontiguousarray(cc.reshape(KT, 128, 2).transpose(1, 0, 2))
    m["ada_w"] = inp["ada_w"]
    m["ada_b"] = np.ascontiguousarray(inp["ada_b"].reshape(2, 48, 128).transpose(0, 2, 1))
    m["gmix"] = np.ascontiguousarray(inp["norm_mix_g"].reshape(2, KT, 128).transpose(0, 2, 1))
    m["gffn"] = np.ascontiguousarray(inp["norm_ffn_g"].reshape(2, KT, 128).transpose(0, 2, 1))
    m["gfin"] = fm(inp["final_norm_g"])
    m["w_in0"] = inp["even_w_in"][0]
    m["w_out0"] = inp["even_w_out"][0]
    lg = inp["hgrn_lb_logits"]
    m["lblT"] = np.ascontiguousarray(lg.reshape(3, 2, 4, 128).transpose(3, 0, 1, 2))
    m["lblrow"] = np.ascontiguousarray(lg.reshape(1, -1))
    m["onorm"] = np.ascontiguousarray(inp["hgrn_onorm_g"][0].reshape(128, 1))
    m["subln"] = np.ascontiguousarray(inp["diff_subln_g"][0].reshape(128, 1))
    m["dlam"] = np.ascontiguousarray(inp["diff_lambda"][0].T)
    m["router_w"] = np.ascontiguousarray(inp["router_w"].reshape(KT, 128, NE).transpose(1, 0, 2))
    m["router_b"] = np.ascontiguousarray(inp["router_b"].reshape(1, NE))
    if with_moe:
        m["w_gate"] = inp["moe_w_gate"]; m["w_up"] = inp["moe_w_up"]; m["w_down"] = inp["moe_w_down"]
    m["w_qkv1"] = inp["odd_w_qkv"][0]; m["w_out1"] = inp["odd_w_out"][0]
    m["sink"] = np.ascontiguousarray(inp["swa_sink"][0].reshape(1, 16))
    cos, sin = rope_tables(T)
    cosT = np.ones((128, NTOK), np.float32); sinT = np.zeros((128, NTOK), np.float32)
    cosT[0:64, NCTX:] = cos.T; cosT[64:128, NCTX:] = cos.T
    sinT[0:64, NCTX:] = sin.T; sinT[64:128, NCTX:] = sin.T
    m["cosT"] = cosT; m["sinT"] = sinT
    R = np.zeros((128, 128), np.float32)
    for o in (0, 64):
        for i in range(16):
            R[o + i, o + 16 + i] = -1.0; R[o + 16 + i, o + i] = 1.0
            R[o + 32 + i, o + 48 + i] = -1.0; R[o + 48 + i, o + 32 + i] = 1.0
    m["rotm"] = np.ascontiguousarray(R.T)
    s_ = np.arange(128)[:, None]; t_ = np.arange(128)[None, :]
    cs = np.stack([np.eye(128), s_ <= t_, s_ >= t_, s_ > t_, s_ < t_, np.ones((128, 128))], axis=1).astype(np.float32)
    m["consts"] = np.ascontiguousarray(cs)
    return m


_T_FULL = 8192


def kernel(**inputs):
    inp = {k: np.asarray(v) for k, v in inputs.items()}
    T = inp["x"].shape[1]
    nc = build(T)
    maps = [prep_inputs(inp, b, T) for b in range(2)]
    in_maps = [maps[c % 2] for c in range(8)]
    res = run_bass_kernel_spmd(nc, in_maps, core_ids=list(range(8)))
    outs = [np.ascontiguousarray(np.asarray(res.results[b]["out"]).T) for b in range(2)]
    return np.stack(outs, axis=0).astype(np.float32)
```
